# Optimizing a Trainium2 kernel written in Bass

```python
import math
import jax
import jax.numpy as jnp
from jax import lax
import numpy as np

D_MODEL = 1024
BATCH = 16
SEQ = 4096
DEPTH = 4
DEC_BATCH = 16
DEC_SEQ = 16
PAST_LEN = 2048

CHUNK = 64
Q_BLOCK = 128
H_A = 4
DH_A = 64
A_WIDTH = H_A * 2 * DH_A
H_M = 4
DH_M = 128
M_WIDTH = H_M * DH_M
CONV_W = 4
D_FF = 2816
N_MOD = 9
EPS = 1e-6
ALIBI_SLOPES = tuple(2.0 ** (-8.0 * (i + 1) / H_A) for i in range(H_A))

OFF_AQ = 0
OFF_AK = OFF_AQ + A_WIDTH
OFF_AV = OFF_AK + A_WIDTH
OFF_MQ = OFF_AV + A_WIDTH
OFF_MK = OFF_MQ + M_WIDTH
OFF_MV = OFF_MK + M_WIDTH
OFF_MO = OFF_MV + M_WIDTH
OFF_MI = OFF_MO + M_WIDTH
OFF_MF = OFF_MI + H_M
OFF_G = OFF_MF + H_M
IN_WIDTH = OFF_G + 2 * D_MODEL

kernel_name = "streaming_diffattn_mlstm_macaron_step"


def _rmsnorm(x, g):
    xf = x.astype(jnp.float32)
    y = xf * lax.rsqrt(jnp.mean(xf * xf, axis=-1, keepdims=True) + EPS)
    return (y * g.astype(jnp.float32)).astype(x.dtype)


def _swiglu(h, w1, w3, w2):
    return (jax.nn.silu(h @ w1) * (h @ w3)) @ w2


def _causal_conv(u, buf, w, b):
    t = u.shape[1]
    full = jnp.concatenate([buf.astype(u.dtype), u], axis=1)
    y = b
    for j in range(CONV_W):
        y = y + full[:, j:j + t] * w[j]
    return y, full[:, full.shape[1] - (CONV_W - 1):]


def _diff_attn_scores(q, k, v, q_pos, k_pos, mask, lmb):
    s = jnp.einsum("bqhcd,bkhcd->bhcqk", q, k).astype(jnp.float32) * (DH_A ** -0.5)
    slopes = jnp.asarray(ALIBI_SLOPES, jnp.float32)
    dist = jnp.abs(q_pos[:, None] - k_pos[None, :]).astype(jnp.float32)
    s = s - (slopes[:, None, None] * dist)[None, :, None]
    if mask is not None:
        s = jnp.where(mask, s, -jnp.inf)
    p = jax.nn.softmax(s, axis=-1)
    a = p[:, :, 0] - lmb * p[:, :, 1]
    return jnp.einsum("bhqk,bkhe->bqhe", a.astype(v.dtype), v)


def _diff_attn_prompt(q, k, v, lmb):
    bsz, s = q.shape[:2]
    nb = s // Q_BLOCK
    k_pos = jnp.arange(s)
    q_blocks = jnp.moveaxis(q.reshape(bsz, nb, Q_BLOCK, H_A, 2, DH_A), 1, 0)

    def block(args):
        i, qi = args
        q_pos = i * Q_BLOCK + jnp.arange(Q_BLOCK)
        mask = (k_pos[None, :] // CHUNK) <= (q_pos[:, None] // CHUNK)
        return _diff_attn_scores(qi, k, v, q_pos, k_pos, mask, lmb)

    o = lax.map(block, (jnp.arange(nb), q_blocks))
    return jnp.moveaxis(o, 0, 1).reshape(bsz, s, H_A, 2 * DH_A)


def _diff_attn_sample(q, k, v, past_k, past_v, lmb):
    bsz, t = q.shape[:2]
    p = past_k.shape[1]
    k_all = jnp.concatenate([past_k.astype(k.dtype).reshape(bsz, p, H_A, 2, DH_A), k], axis=1)
    v_all = jnp.concatenate([past_v.astype(v.dtype), v], axis=1)
    k_pos = jnp.arange(p + t)
    q_pos = p + jnp.arange(t)
    return _diff_attn_scores(q, k_all, v_all, q_pos, k_pos, None, lmb)


def _mlstm(q, k, v, log_i, log_f, c0, n0, m0, blk):
    bsz, t = q.shape[:2]
    nc = t // blk

    def chunks(a):
        a = a.astype(jnp.float32).reshape(bsz, nc, blk, *a.shape[2:])
        return jnp.moveaxis(jnp.moveaxis(a, 1, 0), 2, 3)

    causal = jnp.tril(jnp.ones((blk, blk), dtype=bool))

    def step(carry, xs):
        c, n, m = carry
        qc, kc, vc, li, lf = xs
        bcum = jnp.cumsum(lf, axis=-1)
        dmat = jnp.where(causal, bcum[..., :, None] - bcum[..., None, :] + li[..., None, :], -jnp.inf)
        inter = bcum + m[..., None]
        mt = jnp.maximum(inter, jnp.max(dmat, axis=-1))
        sw = jnp.einsum("bhtd,bhsd->bhts", qc, kc) * jnp.exp(dmat - mt[..., None])
        w_inter = jnp.exp(inter - mt)
        num = jnp.einsum("bhts,bhse->bhte", sw, vc) + w_inter[..., None] * jnp.einsum("bhed,bhtd->bhte", c, qc)
        den = jnp.sum(sw, axis=-1) + w_inter * jnp.einsum("bhd,bhtd->bht", n, qc)
        h = num / jnp.maximum(jnp.abs(den), jnp.exp(-mt))[..., None]
        b_last = bcum[..., -1]
        g = b_last[..., None] - bcum + li
        m_new = jnp.maximum(b_last + m, jnp.max(g, axis=-1))
        ws = jnp.exp(g - m_new[..., None])
        wc = jnp.exp(b_last + m - m_new)
        c_new = wc[..., None, None] * c + jnp.einsum("bhs,bhse,bhsd->bhed", ws, vc, kc)
        n_new = wc[..., None] * n + jnp.einsum("bhs,bhsd->bhd", ws, kc)
        return (c_new, n_new, m_new), h

    carry0 = (c0.astype(jnp.float32), n0.astype(jnp.float32), m0.astype(jnp.float32))
    (c_f, n_f, m_f), hs = lax.scan(step, carry0, (chunks(q), chunks(k), chunks(v), chunks(log_i), chunks(log_f)))
    hs = jnp.moveaxis(jnp.moveaxis(hs, 0, 1), 2, 3).reshape(bsz, t, H_M, DH_M)
    return hs, (c_f, n_f, m_f)


def _token_mixer(h, l, past, w_in, b_if, conv_w, conv_b, lam, norm_a, norm_m, w_pa, w_pb, w_out):
    bsz, t, _ = h.shape
    z = h @ w_in[l]
    q = z[..., OFF_AQ:OFF_AK].reshape(bsz, t, H_A, 2, DH_A)
    k = z[..., OFF_AK:OFF_AV].reshape(bsz, t, H_A, 2, DH_A)
    v = z[..., OFF_AV:OFF_MQ].reshape(bsz, t, H_A, 2 * DH_A)
    lam_init = 0.8 - 0.6 * math.exp(-0.3 * l)
    lp = lam[l].astype(jnp.float32)
    lmb = jnp.exp(jnp.sum(lp[0] * lp[1])) - jnp.exp(jnp.sum(lp[2] * lp[3])) + lam_init
    if past is None:
        o_a = _diff_attn_prompt(q, k, v, lmb)
        c0 = jnp.zeros((bsz, H_M, DH_M, DH_M), jnp.float32)
        n0 = jnp.zeros((bsz, H_M, DH_M), jnp.float32)
        m0 = jnp.zeros((bsz, H_M), jnp.float32)
        conv0 = jnp.zeros((bsz, CONV_W - 1, 2 * M_WIDTH), h.dtype)
        blk = CHUNK
    else:
        past_k, past_v, c0, n0, m0, conv0 = past
        o_a = _diff_attn_sample(q, k, v, past_k, past_v, lmb)
        blk = t
    o_a = (_rmsnorm(o_a, norm_a[l]) * (1.0 - lam_init)).reshape(bsz, t, A_WIDTH)
    qk, conv_new = _causal_conv(z[..., OFF_MQ:OFF_MV], conv0, conv_w[l], conv_b[l])
    qk = jax.nn.silu(qk)
    qm = qk[..., :M_WIDTH].reshape(bsz, t, H_M, DH_M)
    km = qk[..., M_WIDTH:].reshape(bsz, t, H_M, DH_M) * (DH_M ** -0.5)
    vm = z[..., OFF_MV:OFF_MO].reshape(bsz, t, H_M, DH_M)
    og = jax.nn.sigmoid(z[..., OFF_MO:OFF_MI]).reshape(bsz, t, H_M, DH_M)
    gates = z[..., OFF_MI:OFF_G].astype(jnp.float32) + b_if[l].astype(jnp.float32)
    log_i = gates[..., :H_M]
    log_f = jax.nn.log_sigmoid(gates[..., H_M:])
    hm, (c_new, n_new, m_new) = _mlstm(qm, km, vm, log_i, log_f, c0, n0, m0, blk)
    o_m = _rmsnorm(og * hm.astype(h.dtype), norm_m[l]).reshape(bsz, t, M_WIDTH)
    g = jax.nn.sigmoid(z[..., OFF_G:])
    merged = g[..., :D_MODEL] * (o_a @ w_pa[l]) + g[..., D_MODEL:] * (o_m @ w_pb[l])
    y = merged @ w_out[l]
    k_rows = k.reshape(bsz, t, H_A, 2 * DH_A)
    return y, (k_rows, v, c_new, n_new, m_new, conv_new)


def _trunk(x, c, past, w_ada, b_ada, norm_g, w_ffn1, w_ffn3, w_ffn2, w_in, b_if, conv_w, conv_b,
           lam, norm_a, norm_m, w_pa, w_pb, w_out):
    bsz = x.shape[0]
    new = ([], [], [], [], [], [])
    for l in range(DEPTH):
        mod = (jax.nn.silu(c) @ w_ada[l] + b_ada[l]).reshape(bsz, N_MOD, 1, D_MODEL)
        sh1, sc1, g1, sh2, sc2, g2, sh3, sc3, g3 = [mod[:, i] for i in range(N_MOD)]
        h = _rmsnorm(x, norm_g[l, 0]) * (1.0 + sc1) + sh1
        x = x + 0.5 * g1 * _rmsnorm(_swiglu(h, w_ffn1[l, 0], w_ffn3[l, 0], w_ffn2[l, 0]), norm_g[l, 1])
        h = _rmsnorm(x, norm_g[l, 2]) * (1.0 + sc2) + sh2
        layer_past = None if past is None else tuple(a[l] for a in past)
        y, states = _token_mixer(h, l, layer_past, w_in, b_if, conv_w, conv_b, lam, norm_a, norm_m,
                                 w_pa, w_pb, w_out)
        x = x + g2 * _rmsnorm(y, norm_g[l, 3])
        h = _rmsnorm(x, norm_g[l, 4]) * (1.0 + sc3) + sh3
        x = x + 0.5 * g3 * _rmsnorm(_swiglu(h, w_ffn1[l, 1], w_ffn3[l, 1], w_ffn2[l, 1]), norm_g[l, 5])
        for lst, s in zip(new, states):
            lst.append(s)
    return x, [jnp.stack(s) for s in new]


def setup_inputs(seed: int = 0) -> dict:
    key = jax.random.key(seed)
    ks = jax.random.split(key, 32)
    f32 = jnp.float32

    def nrm(k, shape, scale):
        return scale * jax.random.normal(k, shape, f32)

    h_idx = jnp.arange(H_M, dtype=f32)
    f_bias = jnp.broadcast_to(3.0 + 3.0 * h_idx / (H_M - 1), (DEPTH, H_M)) + nrm(ks[20], (DEPTH, H_M), 0.1)
    b_if = jnp.concatenate([nrm(ks[19], (DEPTH, H_M), 0.1), f_bias], axis=-1)
    return {
        "x_prompt": nrm(ks[0], (BATCH, SEQ, D_MODEL), 1.0),
        "x_sample": nrm(ks[1], (DEC_BATCH, DEC_SEQ, D_MODEL), 1.0),
        "c_prompt": nrm(ks[2], (BATCH, D_MODEL), 1.0),
        "c_sample": nrm(ks[3], (DEC_BATCH, D_MODEL), 1.0),
        "cache_k": nrm(ks[4], (DEPTH, DEC_BATCH, PAST_LEN, H_A, 2 * DH_A), 1.0),
        "cache_v": nrm(ks[5], (DEPTH, DEC_BATCH, PAST_LEN, H_A, 2 * DH_A), 1.0),
        "state_c": nrm(ks[6], (DEPTH, DEC_BATCH, H_M, DH_M, DH_M), 0.1),
        "state_n": nrm(ks[7], (DEPTH, DEC_BATCH, H_M, DH_M), 0.1),
        "state_m": nrm(ks[8], (DEPTH, DEC_BATCH, H_M), 0.5),
        "state_conv": nrm(ks[9], (DEPTH, DEC_BATCH, CONV_W - 1, 2 * M_WIDTH), 1.0),
        "w_ada": nrm(ks[10], (DEPTH, D_MODEL, N_MOD * D_MODEL), 0.5 * D_MODEL ** -0.5),
        "b_ada": nrm(ks[11], (DEPTH, N_MOD * D_MODEL), 0.01),
        "norm_g": 1.0 + nrm(ks[12], (DEPTH, 6, D_MODEL), 0.05),
        "w_ffn1": nrm(ks[13], (DEPTH, 2, D_MODEL, D_FF), D_MODEL ** -0.5),
        "w_ffn3": nrm(ks[14], (DEPTH, 2, D_MODEL, D_FF), D_MODEL ** -0.5),
        "w_ffn2": nrm(ks[15], (DEPTH, 2, D_FF, D_MODEL), D_FF ** -0.5),
        "w_in": nrm(ks[16], (DEPTH, D_MODEL, IN_WIDTH), D_MODEL ** -0.5),
        "b_if": b_if,
        "conv_w": nrm(ks[17], (DEPTH, CONV_W, 2 * M_WIDTH), CONV_W ** -0.5),
        "conv_b": nrm(ks[18], (DEPTH, 2 * M_WIDTH), 0.01),
        "lam": nrm(ks[21], (DEPTH, 4, DH_A), 0.1),
        "norm_a": 1.0 + nrm(ks[22], (DEPTH, 2 * DH_A), 0.05),
        "norm_m": 1.0 + nrm(ks[23], (DEPTH, DH_M), 0.05),
        "w_pa": nrm(ks[24], (DEPTH, A_WIDTH, D_MODEL), A_WIDTH ** -0.5),
        "w_pb": nrm(ks[25], (DEPTH, M_WIDTH, D_MODEL), M_WIDTH ** -0.5),
        "w_out": nrm(ks[26], (DEPTH, D_MODEL, D_MODEL), D_MODEL ** -0.5),
    }


def reference(x_prompt, x_sample, c_prompt, c_sample, cache_k, cache_v, state_c, state_n, state_m,
              state_conv, w_ada, b_ada, norm_g, w_ffn1, w_ffn3, w_ffn2, w_in, b_if, conv_w, conv_b,
              lam, norm_a, norm_m, w_pa, w_pb, w_out):
    y_prompt, (pk, pv, pc, pn, pm, pconv) = _trunk(
        x_prompt, c_prompt, None, w_ada, b_ada, norm_g, w_ffn1, w_ffn3, w_ffn2, w_in, b_if,
        conv_w, conv_b, lam, norm_a, norm_m, w_pa, w_pb, w_out)
    y_sample, (sk, sv, sc, sn, sm, sconv) = _trunk(
        x_sample, c_sample, (cache_k, cache_v, state_c, state_n, state_m, state_conv),
        w_ada, b_ada, norm_g, w_ffn1, w_ffn3, w_ffn2, w_in, b_if,
        conv_w, conv_b, lam, norm_a, norm_m, w_pa, w_pb, w_out)
    return (y_prompt, y_sample, pk, pv, pc, pn, pm, pconv, sk, sv, sc, sn, sm, sconv)
```

```python
import math
from contextlib import ExitStack
import numpy as np
import concourse.bass as bass
import concourse.mybir as mybir
from concourse.bass_utils import run_bass_kernel_spmd

F32 = mybir.dt.float32
BF16 = mybir.dt.bfloat16
AF = mybir.ActivationFunctionType
ALU = mybir.AluOpType
AX = mybir.AxisListType

D = 1024
DEPTH = 4
SEQ = 4096
DEC_SEQ = 16
PAST = 2048
DFF = 2816
NFF = 22
INW = 5640
OFF_AQ, OFF_AK, OFF_AV, OFF_MQ, OFF_MK, OFF_MV, OFF_MO, OFF_MI, OFF_MF, OFF_G = 0, 512, 1024, 1536, 2048, 2560, 3072, 3584, 3588, 3592
EPS = 1e-6
SLOPES = [2.0 ** (-8.0 * (i + 1) / 4) for i in range(4)]
LAM_INIT = [0.8 - 0.6 * math.exp(-0.3 * l) for l in range(DEPTH)]
NEG = -1.0e30

C_ID, C_MN, C_FP, C_QP, C_BK, C_SEL, C_OH, C_END = 0, 128, 192, 704, 1216, 1360, 1872, 2128


def make_consts():
    c = np.zeros((128, C_END), np.float32)
    c[:, C_ID:C_ID + 128] = np.eye(128, dtype=np.float32)
    s = np.arange(64)[:, None]
    t = np.arange(64)[None, :]
    c[0:64, C_MN:C_MN + 64] = np.where(s <= t, 0.0, NEG)
    k = np.arange(128)[:, None].astype(np.float64)
    j = np.arange(512)[None, :].astype(np.float64)
    fp = j - np.abs(j - k) - 1.0e7 * ((k >= 64) & (j < 64))
    c[:, C_FP:C_FP + 512] = fp
    c[:, C_QP:C_QP + 512] = np.broadcast_to(j, (128, 512))
    for h in range(4):
        for r in range(32):
            c[:, C_BK + h * 36 + r] = SLOPES[h] * (k[:, 0] - 128.0 * r)
        for d in range(4):
            c[:, C_BK + h * 36 + 32 + d] = SLOPES[h] * 128.0 * d
    for h in range(4):
        c[h, C_SEL + h * 128:C_SEL + (h + 1) * 128] = 1.0
    c[0:64, C_OH:C_OH + 128] = 1.0
    c[64:128, C_OH + 128:C_OH + 256] = 1.0
    return c


class Sem:
    __slots__ = ("h", "cnt")

    def __init__(self, h):
        self.h = h
        self.cnt = 0


class Tr:
    __slots__ = ("w", "r", "dsem", "name", "excl")

    def __init__(self, name="", excl=False):
        self.excl = excl
        self.w = None
        self.r = {}
        self.dsem = None
        self.name = name


class Eng:
    def __init__(self, name, sem):
        self.name = name
        self.sem = sem
        self.ops = []
        self.known = {}


class KB:
    def __init__(self, nc, es):
        self.nc = nc
        self.es = es
        self.nsem = 0
        self.pe = Eng("pe", self.new_sem())
        self.act = Eng("act", self.new_sem())
        self.dve = Eng("dve", self.new_sem())
        self.pool = Eng("pool", self.new_sem())
        self.sp = Eng("sp", self.new_sem())
        self.out_sems = {}
        self.n_inst = 0

    def new_sem(self):
        self.nsem += 1
        return Sem(self.es.enter_context(self.nc.semaphore(f"s{self.nsem}")))

    def sbuf(self, name, shape, dtype):
        return self.es.enter_context(self.nc.sbuf_tensor(name, list(shape), dtype))

    def psum(self, name, shape, dtype):
        return self.es.enter_context(self.nc.psum_tensor(name, list(shape), dtype))

    def _deps(self, eng, R, W, own=None):
        need = {}

        def add(ev, raw):
            if ev is None:
                return
            sem, val = ev
            if sem is own:
                return
            if sem is eng.sem and not raw:
                return
            if eng.known.get(sem, 0) >= val:
                return
            if need.get(sem, 0) < val:
                need[sem] = val

        for t in R:
            add(t.w, True)
            if t.excl:
                for s, v in t.r.items():
                    add((s, v), False)
        for t in W:
            add(t.w, False)
            for s, v in t.r.items():
                add((s, v), False)
        for s, v in need.items():
            eng.known[s] = v
        return list(need.items())

    def op(self, eng, fn, R=(), W=()):
        waits = self._deps(eng, R, W)
        eng.sem.cnt += 1
        val = eng.sem.cnt
        eng.ops.append((waits, fn, eng.sem, 1))
        for t in R:
            if t.r.get(eng.sem, 0) < val:
                t.r[eng.sem] = val
        for t in W:
            t.w = (eng.sem, val)
            t.r = {}
        self.n_inst += 1

    def dma(self, q, out, in_, R=(), W=(), prim=None, is_output=False, nowaw=False):
        if prim is None:
            prim = W[0] if W else R[0]
        if prim.dsem is None:
            prim.dsem = self.new_sem()
        sem = prim.dsem
        waits = self._deps(q, R, W, own=(sem if nowaw else None))
        sem.cnt += 16
        val = sem.cnt
        q.ops.append((waits, (lambda e, out=out, in_=in_: e.dma_start(out=out, in_=in_)), sem, 16))
        for t in R:
            if t.r.get(sem, 0) < val:
                t.r[sem] = val
        for t in W:
            t.w = (sem, val)
            t.r = {}
        if is_output:
            self.out_sems[sem] = val
        self.n_inst += 1

    def finish(self):
        self.sp.ops.append((list(self.out_sems.items()), None, None, 0))

    def replay(self):
        with self.nc.Block() as block:
            def run(eng, e):
                for waits, fn, sem, inc in eng.ops:
                    for s, v in waits:
                        e.wait_ge(s.h, v)
                    if fn is not None:
                        fn(e).then_inc(sem.h, inc)

            @block.tensor
            def _(e):
                run(self.pe, e)

            @block.scalar
            def _(e):
                run(self.act, e)

            @block.vector
            def _(e):
                run(self.dve, e)

            @block.gpsimd
            def _(e):
                run(self.pool, e)

            @block.sync
            def _(e):
                run(self.sp, e)


def build(cfg):
    NPS = cfg.get("n_pseq", 2)
    NSS = cfg.get("n_sseq", 2)
    SEQL = cfg.get("seq", SEQ)
    NL = cfg.get("depth", DEPTH)
    NTILE = SEQL // 512
    NSQ = NPS + NSS

    nc = bass.Bass("TRN2", target_bir_lowering=False)

    def din(name, shape):
        return nc.dram_tensor(name, list(shape), F32, kind="ExternalInput").ap()

    def dout(name, shape):
        return nc.dram_tensor(name, list(shape), F32, kind="ExternalOutput").ap()

    def dscr(name, shape, dt=BF16):
        return nc.dram_tensor(name, list(shape), dt, kind="Internal").ap()

    xp = din("xp", [NPS, SEQL, D]); xs = din("xs", [NSS, DEC_SEQ, D]); cc = din("cc", [NSQ, D])
    cache_k = din("cache_k", [NL, NSS, PAST, 512]); cache_v = din("cache_v", [NL, NSS, PAST, 512])
    state_c = din("state_c", [NL, NSS, 4, 128, 128]); state_n = din("state_n", [NL, NSS, 4, 128])
    state_m = din("state_m", [NL, NSS, 4]); state_conv = din("state_conv", [NL, NSS, 3, D])
    w_ada = din("w_ada", [NL, D, 9 * D]); b_ada = din("b_ada", [NL, 9 * D]); norm_g = din("norm_g", [NL, 6, D])
    w_ffn1 = din("w_ffn1", [NL, 2, D, DFF]); w_ffn3 = din("w_ffn3", [NL, 2, D, DFF]); w_ffn2 = din("w_ffn2", [NL, 2, DFF, D])
    w_in = din("w_in", [NL, D, INW]); b_if = din("b_if", [NL, 8]); conv_w = din("conv_w", [NL, 4, D]); conv_b = din("conv_b", [NL, D])
    lam = din("lam", [NL, 4, 64]); norm_a = din("norm_a", [NL, 128]); norm_m = din("norm_m", [NL, 128])
    w_pa = din("w_pa", [NL, 512, D]); w_pb = din("w_pb", [NL, 512, D]); w_out = din("w_out", [NL, D, D])
    consts = din("consts", [128, C_END])

    yp = dout("yp", [NPS, SEQL, D]); ys = dout("ys", [NSS, DEC_SEQ, D])
    pk = dout("pk", [NL, NPS, SEQL, 512]); pv = dout("pv", [NL, NPS, SEQL, 512])
    pc = dout("pc", [NL, NPS, 4, 128, 128]); pn = dout("pn", [NL, NPS, 4, 128]); pm = dout("pm", [NL, NPS, 4]); pconv = dout("pconv", [NL, NPS, 3, D])
    sk = dout("sk", [NL, NSS, DEC_SEQ, 512]); sv = dout("sv", [NL, NSS, DEC_SEQ, 512])
    sc = dout("sc", [NL, NSS, 4, 128, 128]); sn = dout("sn", [NL, NSS, 4, 128]); sm = dout("sm", [NL, NSS, 4]); sconv = dout("sconv", [NL, NSS, 3, D])

    wb1 = dscr("wb1", [NL, 2, D, DFF]); wb3 = dscr("wb3", [NL, 2, D, DFF]); wb2 = dscr("wb2", [NL, 2, DFF, D])
    wbin = dscr("wbin", [NL, D, INW]); wbpa = dscr("wbpa", [NL, 512, D]); wbpb = dscr("wbpb", [NL, 512, D]); wbout = dscr("wbout", [NL, D, D])
    kscr = dscr("kscr", [NPS, NL, 4, 128, SEQL]); vscr = dscr("vscr", [NPS, NL, 4, 128, SEQL // 128, 128])

    DBG = cfg.get("dbg", False)
    dbg_out = dout("dbg", [16, 128, 4096]) if DBG else None
    es = ExitStack()
    with es:
        k = KB(nc, es)
        PE, ACT, DVE, POOL, SP = k.pe, k.act, k.dve, k.pool, k.sp

        def mm(out, lhsT, rhs, R, W, start=True, stop=True, sg=False):
            k.op(PE, lambda e: e.matmul(out, lhsT, rhs, start=start, stop=stop, skip_group_check=sg), R, W)

        def trp(out, in_, idn, R, W):
            k.op(PE, lambda e: e.transpose(out, in_, idn), R, W)

        def act(out, in_, func, R, W, scale=None, bias=None):
            kw = {}
            if scale is not None:
                kw["scale"] = scale
            if bias is not None:
                kw["bias"] = bias
            k.op(ACT, lambda e: e.activation(out=out, in_=in_, func=func, **kw), R, W)

        def tt(eng, out, in0, in1, op, R, W):
            k.op(eng, lambda e: e.tensor_tensor(out=out, in0=in0, in1=in1, op=op), R, W)

        def ts(eng, out, in0, s1, op0, R, W, s2=None, op1=None):
            if op1 is None:
                k.op(eng, lambda e: e.tensor_scalar(out=out, in0=in0, scalar1=s1, scalar2=None, op0=op0), R, W)
            else:
                k.op(eng, lambda e: e.tensor_scalar(out=out, in0=in0, scalar1=s1, scalar2=s2, op0=op0, op1=op1), R, W)

        def stt(out, in0, scalar, in1, op0, op1, R, W):
            k.op(DVE, lambda e: e.scalar_tensor_tensor(out=out, in0=in0, scalar=scalar, in1=in1, op0=op0, op1=op1), R, W)

        def cp(eng, out, in_, R, W):
            k.op(eng, lambda e: e.tensor_copy(out, in_), R, W)

        def recip(out, in_, R, W):
            k.op(DVE, lambda e: e.reciprocal(out=out, in_=in_), R, W)

        def memset(eng, ap, v, W):
            k.op(eng, lambda e: e.memset(ap, v), (), W)

        def rmax(out, in_, R, W):
            k.op(DVE, lambda e: e.tensor_reduce(out=out, in_=in_, axis=AX.X, op=ALU.max), R, W)

        tap_state = {"done": set()}

        def tap(i, ap2d, tr, n):
            if not DBG or i in tap_state["done"]:
                return
            tap_state["done"].add(i)
            k.dma(POOL, dbg_out[i][0:ap2d.shape[0], 0:n], ap2d, R=[tr], prim=Tr("tap"), is_output=True)

        class B:
            def __init__(self, name, shape, dt=F32):
                self.t = k.sbuf(name, shape, dt)
                self.T = Tr(name)

        TT = 512
        cst = B("cst", [128, C_END])
        ident = cst.t[:, C_ID:C_ID + 128]
        onesb = B("onesb", [128, 128], BF16)
        oneh = B("oneh", [128, 2, 128], BF16)
        epsb = B("epsb", [128, 1])
        xT = B("xT", [128, 8, TT]); hT = B("hT", [128, 8, TT], BF16); sq = B("sq", [128, 8, TT], BF16)
        yT = B("yT", [128, 8, TT], BF16)
        rbc = B("rbc", [128, TT])
        tmpf = [B(f"tmpf{i}", [128, TT]) for i in range(4)]
        big = B("big", [128, 13312], BF16)
        big2 = B("big2", [128, 12288], BF16)
        NSLOT = 5
        slots = [B(f"slot{i}", [128, 4096], BF16) for i in range(NSLOT)]
        stg = [B(f"stg{i}", [128, 512]) for i in range(3)]
        PT = [B(f"PT{i}", [128, TT], BF16) for i in range(4)]
        tsc = [B(f"tsc{i}", [128, TT]) for i in range(3)]
        bq = B("bq", [128, 2, TT])
        oaT = B("oaT", [128, 4, TT], BF16); omT = B("omT", [128, 4, TT], BF16)
        coef = B("coef", [128, NL, NSQ, 9, 8])
        ngT = B("ngT", [128, NL, 48]); cwT = B("cwT", [128, NL, 32]); cbT = B("cbT", [128, NL, 8])
        nacol = B("nacol", [128, NL]); nmcol = B("nmcol", [128, NL]); nlam = B("nlam", [128, NL])
        bif = B("bif", [4, NL, 2]); nbf = B("nbf", [4, NL])
        CTn = B("CTn", [128, NL, 4, 129]); mstate = B("mstate", [4, NL])
        carry = B("carry", [128, NL, 8, 3]); kmax2 = B("kmax2", [128, NL, 8])
        CTb = [B(f"CTb{i}", [128, 4, 128], BF16) for i in range(2)]
        nbcb = [B(f"nbcb{i}", [128, 4, 128], BF16) for i in range(2)]
        g_negM = B("g_negM", [4, TT]); g_enm = B("g_enm", [4, TT])
        g_mst = B("g_mst", [4, 9]); g_Mend = B("g_Mend", [4, 8]); g_wc = B("g_wc", [4, 8])
        wcbc = B("wcbc", [128, 4, 8]); acol = B("acol", [64, 8, 4]); wscol = B("wscol", [64, 8, 4])
        small = B("small", [128, 8])
        ctok = B("ctok", [128, 512])
        ctokc = B("ctokc", [128, 128])
        scT = B("scT", [128, 8, NSQ], BF16)
        banks = [(k.psum(f"bank{i}", [128, 512], F32), Tr(f"bank{i}", excl=True)) for i in range(8)]
        bstate = {"i": 0, "avail": list(range(8))}

        def nb():
            a = bstate["avail"]
            bstate["i"] = (bstate["i"] + 1) % len(a)
            return banks[a[bstate["i"]]]

        class V:
            def __init__(self, ap, tr):
                self.t = ap
                self.T = tr
        gA = V(tsc[0].t, tsc[0].T); gB = V(tsc[1].t, tsc[1].T); g_t = V(tsc[2].t, tsc[2].T)
        g_a = V(bq.t[:, 0, :], bq.T); g_am = V(bq.t[:, 1, :], bq.T)
        g_sc = V(stg[0].t[0:4, :], stg[0].T); g_wi = V(stg[2].t[0:4, :], stg[2].T); g_ws = V(tmpf[3].t[0:4, :], tmpf[3].T)

        bigf = big.t[:, :].bitcast(F32)
        big2f = big2.t[:, :].bitcast(F32)
        groups = {"ffn": {}, "att": {}, "ml": {}, "io": {}}

        def view(group, name, ap):
            v = V(ap, Tr(name))
            groups[group][name] = v
            return v

        hid = view("ffn", "hid", big.t[:, 0:NFF * TT].rearrange("p (c t) -> p c t", c=NFF))
        pastK = [view("att", f"pastK{i}", big.t[:, i * 3584:(i + 1) * 3584]) for i in range(2)]
        kTc = view("att", "kTc", big.t[:, 7168:9216].rearrange("p (c t) -> p c t", c=4))
        vtok = view("att", "vtok", big.t[:, 9216:11264].rearrange("p (c t) -> p c t", c=4))
        pastV = [view("att", f"pastV{i}", big2.t[:, i * 3584:(i + 1) * 3584].rearrange("p (b e) -> p b e", e=128)) for i in range(2)]
        qpad = view("att", "qpad", big2.t[:, 7168:11264].rearrange("p (h c t) -> p h c t", h=4, c=2))
        qmT = view("ml", "qmT", big.t[:, 0:2048].rearrange("p (c t) -> p c t", c=4))
        kmT = view("ml", "kmT", big.t[:, 2048:4096].rearrange("p (c t) -> p c t", c=4))
        qw = view("ml", "qw", big.t[:, 4096:6144].rearrange("p (c t) -> p c t", c=4))
        ogT = view("ml", "ogT", big.t[:, 6144:8192].rearrange("p (c t) -> p c t", c=4))
        mvtok = view("ml", "mvtok", big.t[:, 8192:8192 + 8 * 4 * 129].rearrange("p (b h e) -> p b h e", b=8, h=4))
        kws = [view("ml", f"kws{i}", big.t[:, 12320 + i * 128:12320 + (i + 1) * 128]) for i in range(2)]
        swT = [view("ml", f"swT{i}", big.t[:, 12576 + i * 64:12576 + (i + 1) * 64]) for i in range(4)]
        ucat = view("ml", "ucat", big2f[:, 0:4 * (TT + 3)].rearrange("p (c t) -> p c t", c=4))
        kmf = view("ml", "kmf", big2f[:, 2064:2064 + 2048].rearrange("p (c t) -> p c t", c=4))
        negMbc = view("ml", "negMbc", big2f[:, 4112:4112 + 1024].rearrange("p (c t) -> p c t", c=2))
        ETf = [view("ml", f"ETf{i}", big2f[:, 5136 + i * 64:5136 + (i + 1) * 64]) for i in range(4)]
        xtok = view("io", "xtok", bigf[:, 0:4096].rearrange("p (b f) -> p b f", f=1024))
        modv = view("io", "modv", big2f[:, 0:72 * NSQ].rearrange("p (c s) -> p c s", s=NSQ))
        merged = sq

        def phase(g):
            evs = {}
            for og, vs in groups.items():
                if og == g:
                    continue
                for v in vs.values():
                    for s, val in v.T.r.items():
                        if evs.get(s, 0) < val:
                            evs[s] = val
                    if v.T.w is not None and evs.get(v.T.w[0], 0) < v.T.w[1]:
                        evs[v.T.w[0]] = v.T.w[1]
            for v in groups[g].values():
                for s, val in evs.items():
                    if v.T.r.get(s, 0) < val:
                        v.T.r[s] = val

        wconv_tr = [Tr(f"wconv{l}") for l in range(NL)]

        def slab(ap2d, nk, c0, n, l):
            return (ap2d[0:nk * 128, c0:c0 + n].rearrange("(c p) n -> p c n", p=128), nk, n, l)

        def plan_layer(l):
            out = []
            for i in range(2):
                ff = []
                for j in range(6):
                    n = 512 if j < 5 else 256
                    ff.append(slab(wb1[l, i], 8, j * 512, n, l))
                    ff.append(slab(wb3[l, i], 8, j * 512, n, l))
                for oc in range(8):
                    ff.append(slab(wb2[l, i], NFF, oc * 128, 128, l))
                out.append(ff)
            mx = []
            for c0 in (OFF_AQ, OFF_AK, OFF_AV, OFF_MQ, OFF_MK, OFF_MV, OFF_MO):
                mx.append(slab(wbin[l], 8, c0, 512, l))
            mx.append(slab(wbin[l], 8, OFF_MI, 8, l))
            mx.append(slab(wbpa[l], 4, 0, 1024, l))
            mx.append(slab(wbin[l], 8, OFF_G, 512, l)); mx.append(slab(wbin[l], 8, OFF_G + 512, 512, l))
            mx.append(slab(wbpb[l], 4, 0, 1024, l))
            mx.append(slab(wbin[l], 8, OFF_G + 1024, 512, l)); mx.append(slab(wbin[l], 8, OFF_G + 1536, 512, l))
            mx.append(slab(wbout[l], 8, 0, 512, l)); mx.append(slab(wbout[l], 8, 512, 512, l))
            return out[0] + mx + out[1]

        plan = []
        for _ in range(NPS * NTILE + NSS):
            for l in range(NL):
                plan.extend(plan_layer(l))
        ws_state = {"issued": 0, "used": 0}

        def ws_next():
            while ws_state["issued"] < min(len(plan), ws_state["used"] + NSLOT - 2):
                i = ws_state["issued"]
                ap3, nk, n, l = plan[i]
                s = slots[i % NSLOT]
                k.dma(SP, s.t[:, 0:nk * n].rearrange("p (c n) -> p c n", c=nk), ap3, R=[wconv_tr[l]], W=[s.T])
                ws_state["issued"] += 1
            i = ws_state["used"]
            ap3, nk, n, l = plan[i]
            s = slots[i % NSLOT]
            ws_state["used"] += 1
            return s.t[:, 0:nk * n].rearrange("p (c n) -> p c n", c=nk), s.T

        k.dma(SP, cst.t[:, :], consts[:, :], W=[cst.T])
        memset(DVE, onesb.t[:, :], 1.0, [onesb.T])
        memset(DVE, epsb.t[:, :], EPS, [epsb.T])
        cp(DVE, oneh.t[:, :, :], cst.t[:, C_OH:C_OH + 256].rearrange("p (c n) -> p c n", c=2), [cst.T], [oneh.T])
        memset(POOL, big.t[:, :], 0.0, [v.T for g in groups.values() for v in g.values()])
        memset(POOL, big2.t[:, :], 0.0, [v.T for g in groups.values() for v in g.values()])

        def conv_weights(l):
            def c2(dst, src, rows, step):
                for r0 in range(0, rows, step):
                    k.dma(POOL, dst[r0:r0 + step, :], src[r0:r0 + step, :], W=[wconv_tr[l]], prim=wconv_tr[l], nowaw=True)
            for i in range(2):
                c2(wb1[l, i], w_ffn1[l, i], D, 256); c2(wb3[l, i], w_ffn3[l, i], D, 256); c2(wb2[l, i], w_ffn2[l, i], DFF, 256)
            c2(wbin[l], w_in[l], D, 128); c2(wbpa[l], w_pa[l], 512, 256); c2(wbpb[l], w_pb[l], 512, 256); c2(wbout[l], w_out[l], D, 256)

        def load_T(src2d, rows):
            k.dma(SP, ctok.t[0:rows, 0:128], src2d, W=[ctok.T])
            bt, btr = nb()
            trp(bt[:, 0:rows], ctok.t[0:rows, 0:128], ident[0:rows, 0:rows], [ctok.T, cst.T], [btr])
            return bt, btr

        conv_weights(0)
        for l in range(NL):
            ngv = norm_g[l].rearrange("a (c p) -> (a c) p", p=128)
            bt, btr = load_T(ngv[0:32, :], 32)
            cp(DVE, ngT.t[:, l, 0:32], bt[:, 0:32], [btr], [ngT.T])
            bt, btr = load_T(ngv[32:48, :], 16)
            cp(DVE, ngT.t[:, l, 32:48], bt[:, 0:16], [btr], [ngT.T])
            bt, btr = load_T(conv_w[l].rearrange("a (c p) -> (a c) p", p=128), 32)
            cp(DVE, cwT.t[:, l, :], bt[:, 0:32], [btr], [cwT.T])
            bt, btr = load_T(conv_b[l].rearrange("(c p) -> c p", p=128), 8)
            cp(DVE, cbT.t[:, l, :], bt[:, 0:8], [btr], [cbT.T])
            k.dma(SP, nacol.t[:, l:l + 1], norm_a[l].rearrange("(p o) -> p o", o=1), W=[nacol.T])
            k.dma(SP, nmcol.t[:, l:l + 1], norm_m[l].rearrange("(p o) -> p o", o=1), W=[nmcol.T])
            k.dma(SP, bif.t[:, l, 0:1], b_if[l, 0:4].rearrange("(h o) -> h o", o=1), W=[bif.T])
            k.dma(SP, bif.t[:, l, 1:2], b_if[l, 4:8].rearrange("(h o) -> h o", o=1), W=[bif.T])
        for l in range(NL):
            ts(DVE, nacol.t[:, l:l + 1], nacol.t[:, l:l + 1], 1.0 - LAM_INIT[l], ALU.mult, [nacol.T], [nacol.T])
        ts(DVE, nbf.t[:, :], bif.t[:, :, 1], -1.0, ALU.mult, [bif.T], [nbf.T])
        for l in range(NL):
            k.dma(SP, ctok.t[0:1, 0:256], lam[l].rearrange("(o a) d -> o (a d)", o=1), W=[ctok.T])
            tt(DVE, ctok.t[0:1, 256:320], ctok.t[0:1, 0:64], ctok.t[0:1, 64:128], ALU.mult, [ctok.T], [ctok.T])
            tt(DVE, ctok.t[0:1, 320:384], ctok.t[0:1, 128:192], ctok.t[0:1, 192:256], ALU.mult, [ctok.T], [ctok.T])
            k.op(DVE, lambda e: e.tensor_reduce(out=ctok.t[0:1, 384:386], in_=ctok.t[0:1, 256:384].rearrange("p (a d) -> p a d", a=2), axis=AX.X, op=ALU.add), [ctok.T], [ctok.T])
            act(ctok.t[0:1, 386:388], ctok.t[0:1, 384:386], AF.Exp, [ctok.T], [ctok.T])
            tt(DVE, ctok.t[0:1, 388:389], ctok.t[0:1, 387:388], ctok.t[0:1, 386:387], ALU.subtract, [ctok.T], [ctok.T])
            ts(DVE, ctok.t[0:1, 389:390], ctok.t[0:1, 388:389], -LAM_INIT[l], ALU.add, [ctok.T], [ctok.T])
            bt, btr = nb()
            mm(bt[:, 0:1], cst.t[0:1, C_SEL:C_SEL + 128], ctok.t[0:1, 389:390], [cst.T, ctok.T], [btr])
            cp(DVE, nlam.t[:, l:l + 1], bt[:, 0:1], [btr], [nlam.T])

        bt, btr = load_T(cc.rearrange("s (c p) -> (s c) p", p=128), NSQ * 8)
        act(scT.t[:, :, :].rearrange("p c s -> p s c"), bt[:, 0:NSQ * 8].rearrange("p (s c) -> p s c", c=8), AF.Silu, [btr], [scT.T])
        phase("io")
        badT = tmpf[0]
        for l in range(NL):
            bt, btr = load_T(b_ada[l].rearrange("(c p) -> c p", p=128), 72)
            cp(DVE, badT.t[:, 0:72], bt[:, 0:72], [btr], [badT.T])
            for j in range(18):
                s = slots[j % NSLOT]
                k.dma(POOL, s.t[:, 0:4096].rearrange("p (c n) -> p c n", c=8), w_ada[l][:, j * 512:(j + 1) * 512].rearrange("(c p) n -> p c n", p=128), W=[s.T])
                for q4 in range(4):
                    ch = j * 4 + q4
                    bt, btr = nb()
                    for kc in range(8):
                        mm(bt[:, 0:NSQ], s.t[:, kc * 512 + q4 * 128: kc * 512 + (q4 + 1) * 128], scT.t[:, kc, :], [s.T, scT.T], [btr], start=(kc == 0), stop=(kc == 7))
                    ts(DVE, modv.t[:, ch, :], bt[:, 0:NSQ], badT.t[:, ch:ch + 1], ALU.add, [btr, badT.T], [modv.T])
            for sq_i in range(NSQ):
                for j in range(3):
                    sh = modv.t[:, (3 * j) * 8:(3 * j) * 8 + 8, sq_i]
                    scl = modv.t[:, (3 * j + 1) * 8:(3 * j + 1) * 8 + 8, sq_i]
                    gg = modv.t[:, (3 * j + 2) * 8:(3 * j + 2) * 8 + 8, sq_i]
                    stt(coef.t[:, l, sq_i, 3 * j, :], scl, 1.0, ngT.t[:, l, (2 * j) * 8:(2 * j) * 8 + 8], ALU.add, ALU.mult, [modv.T, ngT.T], [coef.T])
                    cp(DVE, coef.t[:, l, sq_i, 3 * j + 1, :], sh, [modv.T], [coef.T])
                    stt(coef.t[:, l, sq_i, 3 * j + 2, :], gg, (1.0 if j == 1 else 0.5), ngT.t[:, l, (2 * j + 1) * 8:(2 * j + 1) * 8 + 8], ALU.mult, ALU.mult, [modv.T, ngT.T], [coef.T])
            if l + 1 < NL:
                conv_weights(l + 1)

        def rms_bc(src, srcT, nchunk, T, dst, mean_scale):
            act(sq.t[:, 0:nchunk, 0:T], src, AF.Square, [srcT], [sq.T])
            bt, btr = nb()
            for c in range(nchunk):
                mm(bt[:, 0:T], onesb.t[:, :], sq.t[:, c, 0:T], [onesb.T, sq.T], [btr], start=(c == 0), stop=(c == nchunk - 1))
            act(dst.t[:, 0:T], bt[:, 0:T], AF.Sqrt, [btr, epsb.T], [dst.T], scale=mean_scale, bias=epsb.t[:, 0:1])
            recip(dst.t[:, 0:T], dst.t[:, 0:T], [dst.T], [dst.T])

        def prenorm(l, sqi, j, T):
            rms_bc(xT.t[:, :, 0:T], xT.T, 8, T, rbc, 1.0 / D)
            for c in range(8):
                tm = tmpf[c % 4]
                tt(DVE, tm.t[:, 0:T], xT.t[:, c, 0:T], rbc.t[:, 0:T], ALU.mult, [xT.T, rbc.T], [tm.T])
                act(hT.t[:, c, 0:T], tm.t[:, 0:T], AF.Identity, [tm.T, coef.T], [hT.T],
                    scale=coef.t[:, l, sqi, 3 * j, c:c + 1], bias=coef.t[:, l, sqi, 3 * j + 1, c:c + 1])

        def postnorm(l, sqi, j, T):
            rms_bc(yT.t[:, :, 0:T], yT.T, 8, T, rbc, 1.0 / D)
            for c in range(8):
                tm = tmpf[c % 4]
                tt(DVE, tm.t[:, 0:T], yT.t[:, c, 0:T], rbc.t[:, 0:T], ALU.mult, [yT.T, rbc.T], [tm.T])
                stt(xT.t[:, c, 0:T], tm.t[:, 0:T], coef.t[:, l, sqi, 3 * j + 2, c:c + 1], xT.t[:, c, 0:T], ALU.mult, ALU.add, [tm.T, coef.T, xT.T], [xT.T])

        def ffn(l, sqi, j, T):
            prenorm(l, sqi, j, T)
            tap(1, hT.t[:, :, :].rearrange("p c t -> p (c t)"), hT.T, 4096)
            tap(7, rbc.t[:, :], rbc.T, 512)
            phase("ffn")
            for sj in range(6):
                n = 512 if sj < 5 else 256
                w1, w1t = ws_next()
                w3, w3t = ws_next()
                tap(9, w1.rearrange("p c n -> p (c n)"), w1t, 4096)
                tap(10, w3.rearrange("p c n -> p (c n)"), w3t, 4096)
                for q4 in range(n // 128):
                    ch = sj * 4 + q4
                    ba, bat = nb()
                    bb, bbt = nb()
                    for kc in range(8):
                        mm(ba[:, 0:T], w1[:, kc, q4 * 128:(q4 + 1) * 128], hT.t[:, kc, 0:T], [w1t, hT.T], [bat], start=(kc == 0), stop=(kc == 7))
                    for kc in range(8):
                        mm(bb[:, 0:T], w3[:, kc, q4 * 128:(q4 + 1) * 128], hT.t[:, kc, 0:T], [w3t, hT.T], [bbt], start=(kc == 0), stop=(kc == 7))
                    tm = tmpf[ch % 4]
                    act(tm.t[:, 0:T], ba[:, 0:T], AF.Silu, [bat], [tm.T])
                    tt(DVE, hid.t[:, ch, 0:T], tm.t[:, 0:T], bb[:, 0:T], ALU.mult, [tm.T, bbt], [hid.T])
            for oc in range(8):
                w2, w2t = ws_next()
                bo, bot = nb()
                for kc in range(NFF):
                    mm(bo[:, 0:T], w2[:, kc, :], hid.t[:, kc, 0:T], [w2t, hid.T], [bot], start=(kc == 0), stop=(kc == NFF - 1))
                act(yT.t[:, oc, 0:T], bo[:, 0:T], AF.Copy, [bot], [yT.T])
            tap(2, yT.t[:, :, :].rearrange("p c t -> p (c t)"), yT.T, 4096)
            tap(8, hid.t[:, 0:8, :].rearrange("p c t -> p (c t)"), hid.T, 4096)
            postnorm(l, sqi, j, T)

        kv_tr = [[Tr(f"kv{s}_{l}") for l in range(NL)] for s in range(max(NPS, 1))]
        PTf = [B(f"PTf{i}", [128, 16]) for i in range(4)]
        kst = V(bq.t[:, :, :].rearrange("p a b -> p (a b)").rearrange("p (b e) -> p b e", e=128), bq.T)

        def mixer(l, sqi, T, is_prompt, seq, tj):
            L = 64 if is_prompt else 16
            MB = T // L
            NB = max(1, T // 128)
            tb = min(T, 128)
            last_tile = (not is_prompt) or (tj == NTILE - 1)
            prenorm(l, sqi, 1, T)
            tap(4, hT.t[:, :, :].rearrange("p c t -> p (c t)"), hT.T, 4096)
            tap(6, rbc.t[:, :], rbc.T, 512)
            phase("att")
            memset(POOL, qpad.t[64:128, :, 0, :], 0.0, [qpad.T])
            memset(POOL, qpad.t[0:64, :, 1, :], 0.0, [qpad.T])
            stop_at(21)
            wq, wqt = ws_next()
            for h in range(4):
                bt, btr = nb()
                for kc in range(8):
                    mm(bt[:, 0:T], wq[:, kc, h * 128:(h + 1) * 128], hT.t[:, kc, 0:T], [wqt, hT.T], [btr], start=(kc == 0), stop=(kc == 7))
                act(qpad.t[0:64, h, 0, 0:T], bt[0:64, 0:T], AF.Copy, [btr], [qpad.T], scale=0.125)
                act(qpad.t[64:128, h, 1, 0:T], bt[64:128, 0:T], AF.Copy, [btr], [qpad.T], scale=0.125)
            stop_at(22)
            wk, wkt = ws_next()
            for h in range(4):
                bt, btr = nb()
                for kc in range(8):
                    mm(bt[:, 0:T], wk[:, kc, h * 128:(h + 1) * 128], hT.t[:, kc, 0:T], [wkt, hT.T], [btr], start=(kc == 0), stop=(kc == 7))
                act(kTc.t[:, h, 0:T], bt[:, 0:T], AF.Copy, [btr], [kTc.T])
            kout = pk[l, seq] if is_prompt else sk[l, seq]
            vout = pv[l, seq] if is_prompt else sv[l, seq]
            for b in range(NB):
                bt, btr = nb()
                for kc in range(8):
                    mm(bt[0:tb, :], hT.t[:, kc, b * 128:b * 128 + tb], wk[:, kc, :], [hT.T, wkt], [btr], start=(kc == 0), stop=(kc == 7))
                st = stg[b % 3]
                cp(DVE, st.t[0:tb, :], bt[0:tb, :], [btr], [st.T])
                k.dma(POOL, kout[tj * 512 + b * 128: tj * 512 + b * 128 + tb, :], st.t[0:tb, :], R=[st.T], prim=st.T, is_output=True)
            stop_at(23)
            wv, wvt = ws_next()
            for b in range(NB):
                bt, btr = nb()
                for kc in range(8):
                    mm(bt[0:tb, :], hT.t[:, kc, b * 128:b * 128 + tb], wv[:, kc, :], [hT.T, wvt], [btr], start=(kc == 0), stop=(kc == 7))
                st = stg[(b + 1) % 3]
                cp(DVE, st.t[0:tb, :], bt[0:tb, :], [btr], [st.T])
                if cfg.get("var", 1) != 1:
                    act(vtok.t[0:tb, b, :], bt[0:tb, :], AF.Copy, [btr], [vtok.T])
                else:
                    cp(DVE, vtok.t[0:tb, b, :], st.t[0:tb, :], [st.T], [vtok.T])
                k.dma(POOL, vout[tj * 512 + b * 128: tj * 512 + b * 128 + tb, :], st.t[0:tb, :], R=[st.T], prim=st.T, is_output=True)
            if is_prompt and tj < NTILE - 1:
                for h in range(4):
                    k.dma(POOL, kscr[seq, l, h][:, tj * 512:(tj + 1) * 512], kTc.t[:, h, :], R=[kTc.T], W=[kv_tr[seq][l]], prim=kTc.T)
                    k.dma(POOL, vscr[seq, l, h][:, tj * 4:(tj + 1) * 4, :], vtok.t[:, :, h * 128:(h + 1) * 128], R=[vtok.T], W=[kv_tr[seq][l]], prim=vtok.T)
            stop_at(3)
            if tj == 0:
                memset(DVE, kmax2.t[:, l, :], 0.0, [kmax2.T])
            npast = tj * 4 if is_prompt else PAST // 128
            rbase = npast
            for h in range(4):
                par = h % 2
                pK, pV = pastK[par], pastV[par]
                if is_prompt:
                    if npast > 0:
                        k.dma(POOL, pK.t[:, 0:npast * 128], kscr[seq, l, h][:, 0:npast * 128], R=[kv_tr[seq][l]], W=[pK.T])
                        k.dma(POOL, pV.t[:, 0:npast, :], vscr[seq, l, h][:, 0:npast, :], R=[kv_tr[seq][l]], W=[pV.T])
                else:
                    k.dma(POOL, pV.t[:, 0:npast, :], cache_v[l, seq][:, h * 128:(h + 1) * 128].rearrange("(b p) e -> p b e", p=128), W=[pV.T])
                    for half in range(2):
                        k.dma(SP, kst.t[:, :, :], cache_k[l, seq][half * 1024:(half + 1) * 1024, h * 128:(h + 1) * 128].rearrange("(b p) e -> p b e", p=128), W=[kst.T])
                        for b4 in range(2):
                            bt, btr = nb()
                            for b in range(4):
                                trp(bt[:, b * 128:(b + 1) * 128], kst.t[:, b4 * 4 + b, :], ident, [kst.T, cst.T], [btr])
                            c0 = (half * 8 + b4 * 4) * 128
                            act(pK.t[:, c0:c0 + 512], bt[:, :], AF.Copy, [btr], [pK.T])
                ksrcs = [(kTc.t[:, h, 0:T], kTc.T, T)]
                if not is_prompt:
                    ksrcs += [(pK.t[:, g * 512:(g + 1) * 512], pK.T, 512) for g in range(npast // 4)]
                for gi_, (ksrc, ktr, n_) in enumerate(ksrcs):
                    act(sq.t[:, 2 + gi_ % 2, 0:n_], ksrc, AF.Square, [ktr], [sq.T])
                    for c in range(2):
                        bt, btr = nb()
                        mm(bt[:, 0:n_], oneh.t[:, c, :], sq.t[:, 2 + gi_ % 2, 0:n_], [oneh.T, sq.T], [btr])
                        rmax(small.t[:, c:c + 1], bt[:, 0:n_], [btr], [small.T])
                        tt(DVE, kmax2.t[:, l, h * 2 + c:h * 2 + c + 1], kmax2.t[:, l, h * 2 + c:h * 2 + c + 1], small.t[:, c:c + 1], ALU.max, [small.T, kmax2.T], [kmax2.T])
                for c in range(2):
                    act(sq.t[:, c, 0:T], qpad.t[:, h, c, 0:T], AF.Square, [qpad.T], [sq.T])
                    bt, btr = nb()
                    mm(bt[:, 0:T], onesb.t[:, :], sq.t[:, c, 0:T], [onesb.T, sq.T], [btr])
                    tm = tmpf[c]
                    act(tm.t[:, 0:T], bt[:, 0:T], AF.Sqrt, [btr, kmax2.T], [tm.T], scale=kmax2.t[:, l, h * 2 + c:h * 2 + c + 1])
                    stt(bq.t[:, c, 0:T], cst.t[:, C_QP:C_QP + T], -SLOPES[h], tm.t[:, 0:T], ALU.mult, ALU.subtract, [cst.T, tm.T], [bq.T])
                acc = [banks[i] for i in (0, 1, 2, 3)]
                bstate["avail"] = [4, 5, 6, 7]
                nblk = npast + NB
                for kb in range(nblk):
                    diag = kb >= npast
                    d = kb - npast
                    q0 = d * 128 if diag else 0
                    nk = tb if diag else 128
                    for c in range(2):
                        sb_, sbt = nb()
                        if diag:
                            mm(sb_[0:nk, q0:T], kTc.t[:, h, d * 128:d * 128 + nk], qpad.t[:, h, c, q0:T], [kTc.T, qpad.T], [sbt])
                        else:
                            mm(sb_[0:nk, q0:T], pK.t[:, kb * 128:(kb + 1) * 128], qpad.t[:, h, c, q0:T], [pK.T, qpad.T], [sbt])
                        tsb = tsc[(kb * 2 + c) % 3]
                        if diag:
                            stt(tsb.t[0:nk, q0:T], cst.t[0:nk, C_FP:C_FP + T - q0], SLOPES[h], sb_[0:nk, q0:T], ALU.mult, ALU.add, [cst.T, sbt], [tsb.T])
                            tt(DVE, tsb.t[0:nk, q0:T], tsb.t[0:nk, q0:T], bq.t[0:nk, c, q0:T], ALU.add, [tsb.T, bq.T], [tsb.T])
                            bcol = cst.t[0:nk, C_BK + h * 36 + 32 + d:C_BK + h * 36 + 33 + d]
                        else:
                            tt(DVE, tsb.t[0:nk, q0:T], sb_[0:nk, q0:T], bq.t[0:nk, c, q0:T], ALU.add, [sbt, bq.T], [tsb.T])
                            r = rbase - kb
                            bcol = cst.t[0:nk, C_BK + h * 36 + r:C_BK + h * 36 + r + 1]
                        p = PT[(kb * 2 + c) % 4]
                        act(p.t[0:nk, q0:T], tsb.t[0:nk, q0:T], AF.Exp, [tsb.T, cst.T], [p.T], bias=bcol)
                        first = (kb == 0)
                        last = (kb == nblk - 1)
                        if diag and not is_prompt:
                            mm(acc[c][0][:, T:2 * T], vtok.t[0:nk, d, h * 128:(h + 1) * 128], p.t[0:nk, 0:T], [vtok.T, p.T], [acc[c][1]], start=True, stop=True, sg=True)
                            mm(acc[2 + c][0][:, T:2 * T], onesb.t[0:nk, :], p.t[0:nk, 0:T], [onesb.T, p.T], [acc[2 + c][1]], start=True, stop=True, sg=True)
                            continue
                        if not is_prompt:
                            last = (kb == npast - 1)
                        if diag:
                            mm(acc[c][0][:, q0:T], vtok.t[0:nk, d, h * 128:(h + 1) * 128], p.t[0:nk, q0:T], [vtok.T, p.T], [acc[c][1]], start=first, stop=last, sg=True)
                        else:
                            mm(acc[c][0][:, q0:T], pV.t[:, kb, :], p.t[0:nk, q0:T], [pV.T, p.T], [acc[c][1]], start=first, stop=last, sg=True)
                        mm(acc[2 + c][0][:, q0:T], onesb.t[0:nk, :], p.t[0:nk, q0:T], [onesb.T, p.T], [acc[2 + c][1]], start=first, stop=last, sg=True)
                bstate["avail"] = list(range(8))
                r0, r1, o0 = tmpf[0], tmpf[1], tmpf[2]
                if is_prompt:
                    srcs = [acc[i][0][:, 0:T] for i in range(4)]
                    srct = [acc[i][1] for i in range(4)]
                else:
                    srcs, srct = [], []
                    for i in range(4):
                        cp(DVE, tsc[i % 3].t[:, 0:T], acc[i][0][:, 0:T], [acc[i][1]], [tsc[i % 3].T])
                        dsti = PTf[i]
                        tt(DVE, dsti.t[:, 0:T], tsc[i % 3].t[:, 0:T], acc[i][0][:, T:2 * T], ALU.add, [tsc[i % 3].T, acc[i][1]], [dsti.T])
                        srcs.append(dsti.t[:, 0:T]); srct.append(dsti.T)
                recip(r0.t[:, 0:T], srcs[2], [srct[2]], [r0.T])
                recip(r1.t[:, 0:T], srcs[3], [srct[3]], [r1.T])
                tt(DVE, o0.t[:, 0:T], srcs[0], r0.t[:, 0:T], ALU.mult, [srct[0], r0.T], [o0.T])
                tt(DVE, r1.t[:, 0:T], srcs[1], r1.t[:, 0:T], ALU.mult, [srct[1], r1.T], [r1.T])
                stt(o0.t[:, 0:T], r1.t[:, 0:T], nlam.t[:, l:l + 1], o0.t[:, 0:T], ALU.mult, ALU.add, [r1.T, nlam.T, o0.T], [o0.T])
                rms_bc(o0.t[:, 0:T].rearrange("p (c t) -> p c t", c=1), o0.T, 1, T, r0, 1.0 / 128)
                stt(oaT.t[:, h, 0:T], o0.t[:, 0:T], nacol.t[:, l:l + 1], r0.t[:, 0:T], ALU.mult, ALU.mult, [o0.T, nacol.T, r0.T], [oaT.T])

            stop_at(4)
            phase("ml")
            if is_prompt:
                if tj == 0:
                    memset(DVE, carry.t[:, l, :, :], 0.0, [carry.T])
            else:
                for half in range(2):
                    k.dma(SP, ctok.t[0:3, :], state_conv[l, seq][:, half * 512:(half + 1) * 512], W=[ctok.T])
                    bt, btr = nb()
                    for c4 in range(4):
                        trp(bt[:, c4 * 3:(c4 + 1) * 3], ctok.t[0:3, c4 * 128:(c4 + 1) * 128], ident[0:3, 0:3], [ctok.T, cst.T], [btr])
                    cp(DVE, carry.t[:, l, half * 4:half * 4 + 4, :], bt[:, 0:12].rearrange("p (c j) -> p c j", j=3), [btr], [carry.T])
            for half in range(2):
                wmq, wmqt = ws_next()
                cp(DVE, ucat.t[:, :, 0:3], carry.t[:, l, half * 4:half * 4 + 4, :], [carry.T], [ucat.T])
                for h in range(4):
                    bt, btr = nb()
                    for kc in range(8):
                        mm(bt[:, 0:T], wmq[:, kc, h * 128:(h + 1) * 128], hT.t[:, kc, 0:T], [wmqt, hT.T], [btr], start=(kc == 0), stop=(kc == 7))
                    act(ucat.t[:, h, 3:3 + T], bt[:, 0:T], AF.Copy, [btr], [ucat.T])
                for h in range(4):
                    cch = half * 4 + h
                    tm = tmpf[h]
                    act(tm.t[:, 0:T], ucat.t[:, h, 3:3 + T], AF.Identity, [ucat.T, cwT.T, cbT.T], [tm.T],
                        scale=cwT.t[:, l, 3 * 8 + cch:3 * 8 + cch + 1], bias=cbT.t[:, l, cch:cch + 1])
                    for jj in range(3):
                        stt(tm.t[:, 0:T], ucat.t[:, h, jj:jj + T], cwT.t[:, l, jj * 8 + cch:jj * 8 + cch + 1], tm.t[:, 0:T], ALU.mult, ALU.add, [ucat.T, cwT.T, tm.T], [tm.T])
                    if half == 0:
                        act(qmT.t[:, h, 0:T], tm.t[:, 0:T], AF.Silu, [tm.T], [qmT.T])
                    else:
                        act(kmf.t[:, h, 0:T], tm.t[:, 0:T], AF.Silu, [tm.T], [kmf.T])
                        ts(DVE, kmf.t[:, h, 0:T], kmf.t[:, h, 0:T], 128.0 ** -0.5, ALU.mult, [kmf.T], [kmf.T])
                        cp(DVE, kmT.t[:, h, 0:T], kmf.t[:, h, 0:T], [kmf.T], [kmT.T])
                cp(DVE, carry.t[:, l, half * 4:half * 4 + 4, :], ucat.t[:, :, T:T + 3], [ucat.T], [carry.T])
                if last_tile:
                    cvo = pconv[l, seq] if is_prompt else sconv[l, seq]
                    bt, btr = nb()
                    for h in range(4):
                        mm(bt[0:3, h * 128:(h + 1) * 128], ucat.t[:, h, T:T + 3], ident, [ucat.T, cst.T], [btr])
                    st = stg[half]
                    cp(DVE, st.t[0:3, :], bt[0:3, :], [btr], [st.T])
                    k.dma(POOL, cvo[:, half * 512:(half + 1) * 512], st.t[0:3, :], R=[st.T], prim=st.T, is_output=True)
            wmv, wmvt = ws_next()
            memset(POOL, mvtok.t[:, :, :, :].rearrange("p b h e -> p (b h e)"), 0.0, [mvtok.T])
            for sw_ in swT:
                memset(POOL, sw_.t[:, :], 0.0, [sw_.T])
            for c in range(MB):
                bt, btr = nb()
                for kc in range(8):
                    mm(bt[0:L, :], hT.t[:, kc, c * L:(c + 1) * L], wmv[:, kc, :], [hT.T, wmvt], [btr], start=(kc == 0), stop=(kc == 7))
                act(mvtok.t[0:L, c, :, 0:128], bt[0:L, :].rearrange("p (h e) -> p h e", h=4), AF.Copy, [btr], [mvtok.T])
            for c in range(MB):
                memset(DVE, mvtok.t[0:L, c, :, 128:129], 1.0, [mvtok.T])

            wmo, wmot = ws_next()
            for h in range(4):
                bt, btr = nb()
                for kc in range(8):
                    mm(bt[:, 0:T], wmo[:, kc, h * 128:(h + 1) * 128], hT.t[:, kc, 0:T], [wmot, hT.T], [btr], start=(kc == 0), stop=(kc == 7))
                act(ogT.t[:, h, 0:T], bt[:, 0:T], AF.Sigmoid, [btr], [ogT.T])
            wg, wgt = ws_next()
            gi, git = nb()
            for kc in range(8):
                mm(gi[0:4, 0:T], wg[:, kc, 0:4], hT.t[:, kc, 0:T], [wgt, hT.T], [git], start=(kc == 0), stop=(kc == 7))
            gf, gft = nb()
            for kc in range(8):
                mm(gf[0:4, 0:T], wg[:, kc, 4:8], hT.t[:, kc, 0:T], [wgt, hT.T], [gft], start=(kc == 0), stop=(kc == 7))

            stop_at(5)
            def v3(bf, lo=0, hi=None):
                hi = L if hi is None else hi
                return bf.t[0:4, 0:T].rearrange("p (c t) -> p c t", t=L)[:, :, lo:hi]

            def scan(a, b, op):
                sh = 1
                while sh < L:
                    tt(DVE, v3(b, sh, L), v3(a, sh, L), v3(a, 0, L - sh), op, [a.T], [b.T])
                    cp(DVE, v3(b, 0, sh), v3(a, 0, sh), [a.T], [b.T])
                    a, b = b, a
                    sh *= 2
                return a

            act(gA.t[0:4, 0:T], gf[0:4, 0:T], AF.Exp, [gft, nbf.T], [gA.T], scale=-1.0, bias=nbf.t[:, l:l + 1])
            act(gA.t[0:4, 0:T], gA.t[0:4, 0:T], AF.Ln, [gA.T], [gA.T], bias=1.0)
            res = scan(gA, gB, ALU.add)
            cp(DVE, g_sc.t[:, 0:T], res.t[0:4, 0:T], [res.T], [g_sc.T])
            stt(g_a.t[0:4, 0:T], gi[0:4, 0:T], bif.t[:, l, 0:1], g_sc.t[:, 0:T], ALU.add, ALU.add, [git, bif.T, g_sc.T], [g_a.T])
            cp(DVE, gA.t[0:4, 0:T], g_a.t[0:4, 0:T], [g_a.T], [gA.T])
            res = scan(gA, gB, ALU.max)
            cp(DVE, g_am.t[0:4, 0:T], res.t[0:4, 0:T], [res.T], [g_am.T])
            if is_prompt:
                if tj == 0:
                    memset(DVE, mstate.t[:, l:l + 1], 0.0, [mstate.T])
            else:
                k.dma(SP, mstate.t[:, l:l + 1], state_m[l, seq].rearrange("(h o) -> h o", o=1), W=[mstate.T])
            cp(DVE, g_mst.t[:, 0:1], mstate.t[:, l:l + 1], [mstate.T], [g_mst.T])
            am_end = v3(g_am, L - 1, L)
            sc_end = v3(g_sc, L - 1, L)
            for c in range(MB):
                stt(g_mst.t[:, c + 1:c + 2], g_mst.t[:, c:c + 1], am_end[:, c, :], sc_end[:, c, :], ALU.max, ALU.subtract, [g_mst.T, g_am.T, g_sc.T], [g_mst.T])
            cp(DVE, mstate.t[:, l:l + 1], g_mst.t[:, MB:MB + 1], [g_mst.T], [mstate.T])
            tt(DVE, g_Mend.t[:, 0:MB].unsqueeze(2), g_mst.t[:, 0:MB].unsqueeze(2), am_end, ALU.max, [g_mst.T, g_am.T], [g_Mend.T])
            mst_b = g_mst.t[:, 0:MB].unsqueeze(2).broadcast_to([4, MB, L])
            mend_b = g_Mend.t[:, 0:MB].unsqueeze(2).broadcast_to([4, MB, L])
            tt(DVE, v3(g_t), v3(g_am), mst_b, ALU.max, [g_am.T, g_mst.T], [g_t.T])
            ts(DVE, g_negM.t[:, 0:T], g_t.t[0:4, 0:T], -1.0, ALU.mult, [g_t.T], [g_negM.T])
            tt(DVE, v3(g_t), v3(g_negM), mst_b, ALU.add, [g_negM.T, g_mst.T], [g_t.T])
            act(g_wi.t[:, 0:T], g_t.t[0:4, 0:T], AF.Exp, [g_t.T], [g_wi.T])
            tt(DVE, g_t.t[0:4, 0:T], g_sc.t[:, 0:T], g_negM.t[:, 0:T], ALU.add, [g_sc.T, g_negM.T], [g_t.T])
            act(g_enm.t[:, 0:T], g_t.t[0:4, 0:T], AF.Exp, [g_t.T], [g_enm.T])
            tt(DVE, v3(g_t), v3(g_a), mend_b, ALU.subtract, [g_a.T, g_Mend.T], [g_t.T])
            act(g_ws.t[:, 0:T], g_t.t[0:4, 0:T], AF.Exp, [g_t.T], [g_ws.T])
            tt(DVE, g_wc.t[:, 0:MB], g_mst.t[:, 0:MB], g_Mend.t[:, 0:MB], ALU.subtract, [g_mst.T, g_Mend.T], [g_wc.T])
            act(g_wc.t[:, 0:MB], g_wc.t[:, 0:MB], AF.Exp, [g_wc.T], [g_wc.T])
            if last_tile:
                mo_ = pm[l, seq] if is_prompt else sm[l, seq]
                k.dma(POOL, mo_.rearrange("(h o) -> h o", o=1), mstate.t[:, l:l + 1], R=[mstate.T], prim=mstate.T, is_output=True)
            bta, btar = nb()
            btw, btwr = nb()
            for c in range(MB):
                trp(bta[0:L, c * 4:(c + 1) * 4], g_a.t[0:4, c * L:(c + 1) * L], ident[0:4, 0:4], [g_a.T, cst.T], [btar])
                trp(btw[0:L, c * 4:(c + 1) * 4], g_ws.t[0:4, c * L:(c + 1) * L], ident[0:4, 0:4], [g_ws.T, cst.T], [btwr])
            cp(DVE, acol.t[0:L, 0:MB, :], bta[0:L, 0:MB * 4].rearrange("p (c h) -> p c h", h=4), [btar], [acol.T])
            cp(DVE, wscol.t[0:L, 0:MB, :], btw[0:L, 0:MB * 4].rearrange("p (c h) -> p c h", h=4), [btwr], [wscol.T])
            for h in range(4):
                selh = cst.t[0:4, C_SEL + h * 128:C_SEL + (h + 1) * 128]
                bt, btr = nb()
                mm(bt[:, 0:T], selh, g_wi.t[:, 0:T], [cst.T, g_wi.T], [btr])
                tt(DVE, qw.t[:, h, 0:T], qmT.t[:, h, 0:T], bt[:, 0:T], ALU.mult, [qmT.T, btr], [qw.T])
                bt, btr = nb()
                mm(bt[:, 0:MB], selh, g_wc.t[:, 0:MB], [cst.T, g_wc.T], [btr])
                cp(DVE, wcbc.t[:, h, 0:MB], bt[:, 0:MB], [btr], [wcbc.T])
            stop_at(6)
            if is_prompt:
                if tj == 0:
                    memset(DVE, CTn.t[:, l, :, :], 0.0, [CTn.T])
            else:
                for h in range(4):
                    k.dma(SP, ctokc.t[:, :], state_c[l, seq, h], W=[ctokc.T])
                    bt, btr = nb()
                    trp(bt[:, 0:128], ctokc.t[:, :], ident, [ctokc.T, cst.T], [btr])
                    cp(DVE, CTn.t[:, l, h, 0:128], bt[:, 0:128], [btr], [CTn.T])
                k.dma(SP, ctok.t[0:4, 0:128], state_n[l, seq], W=[ctok.T])
                bt, btr = nb()
                trp(bt[:, 0:4], ctok.t[0:4, 0:128], ident[0:4, 0:4], [ctok.T, cst.T], [btr])
                cp(DVE, CTn.t[:, l, :, 128:129], bt[:, 0:4].unsqueeze(2), [btr], [CTn.T])
            for hp in range(2):
                heads = (2 * hp, 2 * hp + 1)
                numb = {heads[0]: banks[0], heads[1]: banks[1]}
                denb = {heads[0]: banks[2], heads[1]: banks[3]}
                bstate["avail"] = [4, 5, 6, 7]
                for hi_, h in enumerate(heads):
                    selh = cst.t[0:4, C_SEL + h * 128:C_SEL + (h + 1) * 128]
                    bt, btr = nb()
                    mm(bt[:, 0:T], selh, g_negM.t[:, 0:T], [cst.T, g_negM.T], [btr])
                    tt(DVE, negMbc.t[0:L, hi_, 0:T].rearrange("p (c t) -> p c t", t=L), bt[0:L, 0:T].rearrange("p (c t) -> p c t", t=L),
                       cst.t[0:L, C_MN:C_MN + L].unsqueeze(1).broadcast_to([L, MB, L]), ALU.add, [btr, cst.T], [negMbc.T])
                for c in range(MB):
                    par = c % 2
                    cs = slice(c * L, (c + 1) * L)
                    for h in heads:
                        act(CTb[par].t[:, h, :], CTn.t[:, l, h, 0:128], AF.Copy, [CTn.T], [CTb[par].T])
                        cp(DVE, nbcb[par].t[:, h, :], CTn.t[:, l, h, 128:129].broadcast_to([128, 128]), [CTn.T], [nbcb[par].T])
                    for hi_, h in enumerate(heads):
                        i4 = (c * 2 + hi_) % 4
                        sb_, sbt = nb()
                        mm(sb_[0:L, 0:L], kmT.t[:, h, cs], qmT.t[:, h, cs], [kmT.T, qmT.T], [sbt])
                        et = ETf[i4]
                        act(et.t[0:L, 0:L], negMbc.t[0:L, hi_, cs], AF.Exp, [negMbc.T, acol.T], [et.T], bias=acol.t[0:L, c, h:h + 1])
                        sw = swT[i4]
                        tt(DVE, sw.t[0:L, 0:L], sb_[0:L, 0:L], et.t[0:L, 0:L], ALU.mult, [sbt, et.T], [sw.T])
                        kt_, ktt = nb()
                        trp(kt_[0:L, 0:128], kmf.t[:, h, cs], ident, [kmf.T, cst.T], [ktt])
                        kw_ = kws[i4 % 2]
                        ts(DVE, kw_.t[0:L, :], kt_[0:L, 0:128], wscol.t[0:L, c, h:h + 1], ALU.mult, [ktt, wscol.T], [kw_.T])
                        nbk, nbt = numb[h]
                        mm(nbk[:, cs], mvtok.t[:, c, h, 0:128], sw.t[:, 0:L], [mvtok.T, sw.T], [nbt], start=True, stop=False, sg=True)
                        mm(nbk[:, cs], CTb[par].t[:, h, :], qw.t[:, h, cs], [CTb[par].T, qw.T], [nbt], start=False, stop=True, sg=True)
                        dk, dt_ = denb[h]
                        mm(dk[:, cs], onesb.t[:, :], sw.t[:, 0:L], [onesb.T, sw.T], [dt_], start=True, stop=False, sg=True)
                        mm(dk[:, cs], nbcb[par].t[:, h, :], qw.t[:, h, cs], [nbcb[par].T, qw.T], [dt_], start=False, stop=True, sg=True)
                        ub, ubt = nb()
                        mm(ub[:, 0:129], kw_.t[0:L, :], mvtok.t[0:L, c, h, :], [kw_.T, mvtok.T], [ubt])
                        stt(CTn.t[:, l, h, :], CTn.t[:, l, h, :], wcbc.t[:, h, c:c + 1], ub[:, 0:129], ALU.mult, ALU.add, [CTn.T, wcbc.T, ubt], [CTn.T])
                for h in heads:
                    selh = cst.t[0:4, C_SEL + h * 128:C_SEL + (h + 1) * 128]
                    bt, btr = nb()
                    mm(bt[:, 0:T], selh, g_enm.t[:, 0:T], [cst.T, g_enm.T], [btr])
                    e_ = tmpf[0]
                    cp(DVE, e_.t[:, 0:T], bt[:, 0:T], [btr], [e_.T])
                    dk, dt_ = denb[h]
                    d_ = tmpf[1]
                    act(d_.t[:, 0:T], dk[:, 0:T], AF.Abs, [dt_], [d_.T])
                    tt(DVE, d_.t[:, 0:T], d_.t[:, 0:T], e_.t[:, 0:T], ALU.max, [d_.T, e_.T], [d_.T])
                    recip(d_.t[:, 0:T], d_.t[:, 0:T], [d_.T], [d_.T])
                    nbk, nbt = numb[h]
                    hm = tmpf[2]
                    tt(DVE, hm.t[:, 0:T], nbk[:, 0:T], d_.t[:, 0:T], ALU.mult, [nbt, d_.T], [hm.T])
                    tt(DVE, hm.t[:, 0:T], hm.t[:, 0:T], ogT.t[:, h, 0:T], ALU.mult, [hm.T, ogT.T], [hm.T])
                    rms_bc(hm.t[:, 0:T].rearrange("p (c t) -> p c t", c=1), hm.T, 1, T, e_, 1.0 / 128)
                    stt(omT.t[:, h, 0:T], hm.t[:, 0:T], nmcol.t[:, l:l + 1], e_.t[:, 0:T], ALU.mult, ALU.mult, [hm.T, nmcol.T, e_.T], [omT.T])
            bstate["avail"] = list(range(8))
            if last_tile:
                co = pc[l, seq] if is_prompt else sc[l, seq]
                no = pn[l, seq] if is_prompt else sn[l, seq]
                for h in range(4):
                    bt, btr = nb()
                    trp(bt[:, 0:128], CTn.t[:, l, h, 0:128], ident, [CTn.T, cst.T], [btr])
                    st = stg[h % 3]
                    cp(DVE, st.t[:, 0:128], bt[:, 0:128], [btr], [st.T])
                    k.dma(POOL, co[h], st.t[:, 0:128], R=[st.T], prim=st.T, is_output=True)
                bt, btr = nb()
                for h in range(4):
                    mm(bt[0:1, h * 128:(h + 1) * 128], CTn.t[:, l, h, 128:129], ident, [CTn.T, cst.T], [btr])
                st = stg[1]
                cp(DVE, st.t[0:1, :], bt[0:1, :], [btr], [st.T])
                k.dma(POOL, no.rearrange("(o h) d -> o (h d)", o=1), st.t[0:1, :], R=[st.T], prim=st.T, is_output=True)
            stop_at(7)
            for ph in range(2):
                src = oaT if ph == 0 else omT
                wp, wpt = ws_next()
                g0 = ws_next()
                g1 = ws_next()
                for oc in range(8):
                    gsl, gslt = (g0 if oc < 4 else g1)
                    bg, bgt = nb()
                    for kc in range(8):
                        mm(bg[:, 0:T], gsl[:, kc, (oc % 4) * 128:(oc % 4 + 1) * 128], hT.t[:, kc, 0:T], [gslt, hT.T], [bgt], start=(kc == 0), stop=(kc == 7))
                    bp, bpt = nb()
                    for kc in range(4):
                        mm(bp[:, 0:T], wp[:, kc, oc * 128:(oc + 1) * 128], src.t[:, kc, 0:T], [wpt, src.T], [bpt], start=(kc == 0), stop=(kc == 3))
                    tm = tmpf[oc % 4]
                    act(tm.t[:, 0:T], bg[:, 0:T], AF.Sigmoid, [bgt], [tm.T])
                    if ph == 0:
                        tt(DVE, merged.t[:, oc, 0:T], tm.t[:, 0:T], bp[:, 0:T], ALU.mult, [tm.T, bpt], [merged.T])
                    else:
                        tt(DVE, tm.t[:, 0:T], tm.t[:, 0:T], bp[:, 0:T], ALU.mult, [tm.T, bpt], [tm.T])
                        tt(DVE, merged.t[:, oc, 0:T], merged.t[:, oc, 0:T], tm.t[:, 0:T], ALU.add, [tm.T, merged.T], [merged.T])
            for half in range(2):
                wo, wot = ws_next()
                for q4 in range(4):
                    oc = half * 4 + q4
                    bo, bot = nb()
                    for kc in range(8):
                        mm(bo[:, 0:T], wo[:, kc, q4 * 128:(q4 + 1) * 128], merged.t[:, kc, 0:T], [wot, merged.T], [bot], start=(kc == 0), stop=(kc == 7))
                    act(yT.t[:, oc, 0:T], bo[:, 0:T], AF.Copy, [bot], [yT.T])
            postnorm(l, sqi, 1, T)

        def run_tile(is_prompt, seq, tj, T):
            sqi = seq if is_prompt else NPS + seq
            src = xp[seq] if is_prompt else xs[seq]
            dst = yp[seq] if is_prompt else ys[seq]
            NB = max(1, T // 128)
            tb = min(T, 128)
            phase("io")
            k.dma(POOL, xtok.t[0:tb, 0:NB, :], src[tj * 512:tj * 512 + T, :].rearrange("(b p) f -> p b f", p=tb), W=[xtok.T])
            for c in range(8):
                bt, btr = nb()
                for b in range(NB):
                    trp(bt[:, b * 128:b * 128 + tb], xtok.t[0:tb, b, c * 128:(c + 1) * 128], ident[0:tb, 0:tb], [xtok.T, cst.T], [btr])
                if c % 2:
                    cp(DVE, xT.t[:, c, 0:T], bt[:, 0:T], [btr], [xT.T])
                else:
                    act(xT.t[:, c, 0:T], bt[:, 0:T], AF.Copy, [btr], [xT.T])
            stop_at(1)
            tap(0, xT.t[:, :, :].rearrange("p c t -> p (c t)"), xT.T, 4096)
            tap(5, coef.t[:, :, :, :, :].rearrange("p a b c d -> p (a b c d)"), coef.T, NL * NSQ * 72)
            for l in range(NL):
                ffn(l, sqi, 0, T)
                tap(3, xT.t[:, :, :].rearrange("p c t -> p (c t)"), xT.T, 4096)
                stop_at(2)
                mixer(l, sqi, T, is_prompt, seq, tj)
                ffn(l, sqi, 2, T)
            phase("io")
            for b in range(NB):
                for half in range(2):
                    bt, btr = nb()
                    for c4 in range(4):
                        c = half * 4 + c4
                        trp(bt[0:tb, c4 * 128:(c4 + 1) * 128], xT.t[:, c, b * 128:b * 128 + tb], ident, [xT.T, cst.T], [btr])
                    cp(DVE, xtok.t[0:tb, b, half * 512:(half + 1) * 512], bt[0:tb, :], [btr], [xtok.T])
            k.dma(POOL, dst[tj * 512:tj * 512 + T, :].rearrange("(b p) f -> p b f", p=tb), xtok.t[0:tb, 0:NB, :], R=[xtok.T], prim=xtok.T, is_output=True)

        class StopBuild(Exception):
            pass

        def stop_at(level):
            if cfg.get("stop", 99) == level:
                raise StopBuild()
        try:
            stop_at(0)
            for seq in range(NPS):
                for tj in range(NTILE):
                    run_tile(True, seq, tj, 512)
            if not cfg.get("skip_sample", False):
                for seq in range(NSS):
                    run_tile(False, seq, 0, DEC_SEQ)
        except StopBuild:
            pass

        k.finish()
        k.replay()
        build.n_inst = k.n_inst
    return nc


_W_NAMES = ["w_ada", "b_ada", "norm_g", "w_ffn1", "w_ffn3", "w_ffn2", "w_in", "b_if", "conv_w", "conv_b", "lam", "norm_a", "norm_m", "w_pa", "w_pb", "w_out"]


def run(inputs, cfg, n_cores=8):
    NPS = cfg.get("n_pseq", 2)
    NSS = cfg.get("n_sseq", 2)
    nc = build(cfg)
    consts = make_consts()
    f = lambda a: np.ascontiguousarray(np.asarray(a, dtype=np.float32))
    wts = {n: f(inputs[n]) for n in _W_NAMES}
    in_maps = []
    for c in range(n_cores):
        ps = slice(c * NPS, (c + 1) * NPS)
        ss = slice(c * NSS, (c + 1) * NSS)
        m = dict(wts)
        m["xp"] = f(inputs["x_prompt"][ps]); m["xs"] = f(inputs["x_sample"][ss])
        m["cc"] = f(np.concatenate([inputs["c_prompt"][ps], inputs["c_sample"][ss]], axis=0))
        m["cache_k"] = f(np.asarray(inputs["cache_k"])[:, ss].reshape(-1, NSS, PAST, 512))
        m["cache_v"] = f(np.asarray(inputs["cache_v"])[:, ss].reshape(-1, NSS, PAST, 512))
        m["state_c"] = f(np.asarray(inputs["state_c"])[:, ss]); m["state_n"] = f(np.asarray(inputs["state_n"])[:, ss])
        m["state_m"] = f(np.asarray(inputs["state_m"])[:, ss]); m["state_conv"] = f(np.asarray(inputs["state_conv"])[:, ss])
        m["consts"] = consts
        in_maps.append(m)
    res = run_bass_kernel_spmd(nc, in_maps, core_ids=list(range(n_cores)))
    R = res.results
    if cfg.get("dbg", False):
        run.dbg = R[0]["dbg"]
    cat0 = lambda n: np.concatenate([r[n] for r in R], axis=0)
    cat1 = lambda n: np.concatenate([r[n] for r in R], axis=1)
    nl = cfg.get("depth", DEPTH)
    sl = cfg.get("seq", SEQ)
    out = (cat0("yp"), cat0("ys"),
           cat1("pk").reshape(nl, -1, sl, 4, 128), cat1("pv").reshape(nl, -1, sl, 4, 128),
           cat1("pc"), cat1("pn"), cat1("pm"), cat1("pconv"),
           cat1("sk").reshape(nl, -1, DEC_SEQ, 4, 128), cat1("sv").reshape(nl, -1, DEC_SEQ, 4, 128),
           cat1("sc"), cat1("sn"), cat1("sm"), cat1("sconv"))
    return tuple(np.ascontiguousarray(o.astype(np.float32)) for o in out)


def kernel(**inputs):
    return run(inputs, {})
```

```python
import math
from contextlib import ExitStack
import numpy as np
import concourse.bass as bass
import concourse.mybir as mybir
from concourse.bass_utils import run_bass_kernel_spmd

F32 = mybir.dt.float32
BF16 = mybir.dt.bfloat16
AF = mybir.ActivationFunctionType
ALU = mybir.AluOpType
AX = mybir.AxisListType

D = 1024
DEPTH = 4
SEQ = 4096
DEC_SEQ = 16
PAST = 2048
DFF = 2816
NFF = 22
INW = 5640
OFF_AQ, OFF_AK, OFF_AV, OFF_MQ, OFF_MK, OFF_MV, OFF_MO, OFF_MI, OFF_MF, OFF_G = 0, 512, 1024, 1536, 2048, 2560, 3072, 3584, 3588, 3592
EPS = 1e-6
SLOPES = [2.0 ** (-8.0 * (i + 1) / 4) for i in range(4)]
LAM_INIT = [0.8 - 0.6 * math.exp(-0.3 * l) for l in range(DEPTH)]
NEG = -1.0e30

C_ID, C_MN, C_FP, C_QP, C_BK, C_SEL, C_OH, C_END = 0, 128, 192, 704, 1216, 1360, 1872, 2128


def make_consts():
    c = np.zeros((128, C_END), np.float32)
    c[:, C_ID:C_ID + 128] = np.eye(128, dtype=np.float32)
    s = np.arange(64)[:, None]
    t = np.arange(64)[None, :]
    c[0:64, C_MN:C_MN + 64] = np.where(s <= t, 0.0, NEG)
    k = np.arange(128)[:, None].astype(np.float64)
    j = np.arange(512)[None, :].astype(np.float64)
    fp = j - np.abs(j - k) - 1.0e7 * ((k >= 64) & (j < 64))
    c[:, C_FP:C_FP + 512] = fp
    c[:, C_QP:C_QP + 512] = np.broadcast_to(j, (128, 512))
    for h in range(4):
        for r in range(32):
            c[:, C_BK + h * 36 + r] = SLOPES[h] * (k[:, 0] - 128.0 * r)
        for d in range(4):
            c[:, C_BK + h * 36 + 32 + d] = SLOPES[h] * 128.0 * d
    for h in range(4):
        c[h, C_SEL + h * 128:C_SEL + (h + 1) * 128] = 1.0
    c[0:64, C_OH:C_OH + 128] = 1.0
    c[64:128, C_OH + 128:C_OH + 256] = 1.0
    return c


class Sem:
    __slots__ = ("h", "cnt")

    def __init__(self, h):
        self.h = h
        self.cnt = 0


class Tr:
    __slots__ = ("w", "r", "dsem", "name", "excl")

    def __init__(self, name="", excl=False):
        self.excl = excl
        self.w = None
        self.r = {}
        self.dsem = None
        self.name = name


class Eng:
    def __init__(self, name, sem):
        self.name = name
        self.sem = sem
        self.ops = []
        self.known = {}


class KB:
    def __init__(self, nc, es):
        self.nc = nc
        self.es = es
        self.nsem = 0
        self.pe = Eng("pe", self.new_sem())
        self.act = Eng("act", self.new_sem())
        self.dve = Eng("dve", self.new_sem())
        self.pool = Eng("pool", self.new_sem())
        self.sp = Eng("sp", self.new_sem())
        self.out_sems = {}
        self.n_inst = 0

    def new_sem(self):
        self.nsem += 1
        return Sem(self.es.enter_context(self.nc.semaphore(f"s{self.nsem}")))

    def sbuf(self, name, shape, dtype):
        return self.es.enter_context(self.nc.sbuf_tensor(name, list(shape), dtype))

    def psum(self, name, shape, dtype):
        return self.es.enter_context(self.nc.psum_tensor(name, list(shape), dtype))

    def _deps(self, eng, R, W, own=None):
        need = {}

        def add(ev, raw):
            if ev is None:
                return
            sem, val = ev
            if sem is own:
                return
            if sem is eng.sem and not raw:
                return
            if eng.known.get(sem, 0) >= val:
                return
            if need.get(sem, 0) < val:
                need[sem] = val

        for t in R:
            add(t.w, True)
            if t.excl:
                for s, v in t.r.items():
                    add((s, v), False)
        for t in W:
            add(t.w, False)
            for s, v in t.r.items():
                add((s, v), False)
        for s, v in need.items():
            eng.known[s] = v
        return list(need.items())

    def op(self, eng, fn, R=(), W=()):
        waits = self._deps(eng, R, W)
        eng.sem.cnt += 1
        val = eng.sem.cnt
        eng.ops.append((waits, fn, eng.sem, 1))
        for t in R:
            if t.r.get(eng.sem, 0) < val:
                t.r[eng.sem] = val
        for t in W:
            t.w = (eng.sem, val)
            t.r = {}
        self.n_inst += 1

    def dma(self, q, out, in_, R=(), W=(), prim=None, is_output=False, nowaw=False):
        if prim is None:
            prim = W[0] if W else R[0]
        if prim.dsem is None:
            prim.dsem = self.new_sem()
        sem = prim.dsem
        waits = self._deps(q, R, W, own=(sem if nowaw else None))
        sem.cnt += 16
        val = sem.cnt
        q.ops.append((waits, (lambda e, out=out, in_=in_: e.dma_start(out=out, in_=in_)), sem, 16))
        for t in R:
            if t.r.get(sem, 0) < val:
                t.r[sem] = val
        for t in W:
            t.w = (sem, val)
            t.r = {}
        if is_output:
            self.out_sems[sem] = val
        self.n_inst += 1

    def finish(self):
        self.sp.ops.append((list(self.out_sems.items()), None, None, 0))

    def replay(self):
        with self.nc.Block() as block:
            def run(eng, e):
                for waits, fn, sem, inc in eng.ops:
                    for s, v in waits:
                        e.wait_ge(s.h, v)
                    if fn is not None:
                        fn(e).then_inc(sem.h, inc)

            @block.tensor
            def _(e):
                run(self.pe, e)

            @block.scalar
            def _(e):
                run(self.act, e)

            @block.vector
            def _(e):
                run(self.dve, e)

            @block.gpsimd
            def _(e):
                run(self.pool, e)

            @block.sync
            def _(e):
                run(self.sp, e)


def build(cfg):
    NPS = cfg.get("n_pseq", 2)
    NSS = cfg.get("n_sseq", 2)
    SEQL = cfg.get("seq", SEQ)
    NL = cfg.get("depth", DEPTH)
    NTILE = SEQL // 512
    NSQ = NPS + NSS

    nc = bass.Bass("TRN2", target_bir_lowering=False)

    def din(name, shape):
        return nc.dram_tensor(name, list(shape), F32, kind="ExternalInput").ap()

    def dout(name, shape):
        return nc.dram_tensor(name, list(shape), F32, kind="ExternalOutput").ap()

    def dscr(name, shape, dt=BF16):
        return nc.dram_tensor(name, list(shape), dt, kind="Internal").ap()

    xp = din("xp", [NPS, SEQL, D]); xs = din("xs", [NSS, DEC_SEQ, D]); cc = din("cc", [NSQ, D])
    cache_k = din("cache_k", [NL, NSS, PAST, 512]); cache_v = din("cache_v", [NL, NSS, PAST, 512])
    state_c = din("state_c", [NL, NSS, 4, 128, 128]); state_n = din("state_n", [NL, NSS, 4, 128])
    state_m = din("state_m", [NL, NSS, 4]); state_conv = din("state_conv", [NL, NSS, 3, D])
    w_ada = din("w_ada", [NL, D, 9 * D]); b_ada = din("b_ada", [NL, 9 * D]); norm_g = din("norm_g", [NL, 6, D])
    w_ffn1 = din("w_ffn1", [NL, 2, D, DFF]); w_ffn3 = din("w_ffn3", [NL, 2, D, DFF]); w_ffn2 = din("w_ffn2", [NL, 2, DFF, D])
    w_in = din("w_in", [NL, D, INW]); b_if = din("b_if", [NL, 8]); conv_w = din("conv_w", [NL, 4, D]); conv_b = din("conv_b", [NL, D])
    lam = din("lam", [NL, 4, 64]); norm_a = din("norm_a", [NL, 128]); norm_m = din("norm_m", [NL, 128])
    w_pa = din("w_pa", [NL, 512, D]); w_pb = din("w_pb", [NL, 512, D]); w_out = din("w_out", [NL, D, D])
    consts = din("consts", [128, C_END])

    yp = dout("yp", [NPS, SEQL, D]); ys = dout("ys", [NSS, DEC_SEQ, D])
    pk = dout("pk", [NL, NPS, SEQL, 512]); pv = dout("pv", [NL, NPS, SEQL, 512])
    pc = dout("pc", [NL, NPS, 4, 128, 128]); pn = dout("pn", [NL, NPS, 4, 128]); pm = dout("pm", [NL, NPS, 4]); pconv = dout("pconv", [NL, NPS, 3, D])
    sk = dout("sk", [NL, NSS, DEC_SEQ, 512]); sv = dout("sv", [NL, NSS, DEC_SEQ, 512])
    sc = dout("sc", [NL, NSS, 4, 128, 128]); sn = dout("sn", [NL, NSS, 4, 128]); sm = dout("sm", [NL, NSS, 4]); sconv = dout("sconv", [NL, NSS, 3, D])

    wb1 = dscr("wb1", [NL, 2, D, DFF]); wb3 = dscr("wb3", [NL, 2, D, DFF]); wb2 = dscr("wb2", [NL, 2, DFF, D])
    wbin = dscr("wbin", [NL, D, INW]); wbpa = dscr("wbpa", [NL, 512, D]); wbpb = dscr("wbpb", [NL, 512, D]); wbout = dscr("wbout", [NL, D, D])
    kscr = dscr("kscr", [NPS, NL, 4, 128, SEQL]); vscr = dscr("vscr", [NPS, NL, 4, 128, SEQL // 128, 128])

    DBG = cfg.get("dbg", False)
    dbg_out = dout("dbg", [16, 128, 4096]) if DBG else None
    es = ExitStack()
    with es:
        k = KB(nc, es)
        PE, ACT, DVE, POOL, SP = k.pe, k.act, k.dve, k.pool, k.sp

        def mm(out, lhsT, rhs, R, W, start=True, stop=True, sg=False):
            k.op(PE, lambda e: e.matmul(out, lhsT, rhs, start=start, stop=stop, skip_group_check=sg), R, W)

        def trp(out, in_, idn, R, W):
            k.op(PE, lambda e: e.transpose(out, in_, idn), R, W)

        def act(out, in_, func, R, W, scale=None, bias=None):
            kw = {}
            if scale is not None:
                kw["scale"] = scale
            if bias is not None:
                kw["bias"] = bias
            k.op(ACT, lambda e: e.activation(out=out, in_=in_, func=func, **kw), R, W)

        def tt(eng, out, in0, in1, op, R, W):
            k.op(eng, lambda e: e.tensor_tensor(out=out, in0=in0, in1=in1, op=op), R, W)

        def ts(eng, out, in0, s1, op0, R, W, s2=None, op1=None):
            if op1 is None:
                k.op(eng, lambda e: e.tensor_scalar(out=out, in0=in0, scalar1=s1, scalar2=None, op0=op0), R, W)
            else:
                k.op(eng, lambda e: e.tensor_scalar(out=out, in0=in0, scalar1=s1, scalar2=s2, op0=op0, op1=op1), R, W)

        def stt(out, in0, scalar, in1, op0, op1, R, W):
            k.op(DVE, lambda e: e.scalar_tensor_tensor(out=out, in0=in0, scalar=scalar, in1=in1, op0=op0, op1=op1), R, W)

        def cp(eng, out, in_, R, W):
            k.op(eng, lambda e: e.tensor_copy(out, in_), R, W)

        def recip(out, in_, R, W):
            k.op(DVE, lambda e: e.reciprocal(out=out, in_=in_), R, W)

        def memset(eng, ap, v, W):
            k.op(eng, lambda e: e.memset(ap, v), (), W)

        def rmax(out, in_, R, W):
            k.op(DVE, lambda e: e.tensor_reduce(out=out, in_=in_, axis=AX.X, op=ALU.max), R, W)

        tap_state = {"done": set()}

        def tap(i, ap2d, tr, n):
            if not DBG or i in tap_state["done"]:
                return
            tap_state["done"].add(i)
            k.dma(POOL, dbg_out[i][0:ap2d.shape[0], 0:n], ap2d, R=[tr], prim=Tr("tap"), is_output=True)

        class B:
            def __init__(self, name, shape, dt=F32):
                self.t = k.sbuf(name, shape, dt)
                self.T = Tr(name)

        TT = 512
        cst = B("cst", [128, C_END])
        ident = cst.t[:, C_ID:C_ID + 128]
        onesb = B("onesb", [128, 128], BF16)
        oneh = B("oneh", [128, 2, 128], BF16)
        epsb = B("epsb", [128, 1])
        xT = B("xT", [128, 8, TT]); hT = B("hT", [128, 8, TT], BF16); sq = B("sq", [128, 8, TT], BF16)
        yT = B("yT", [128, 8, TT], BF16)
        rbc = B("rbc", [128, TT])
        tmpf = [B(f"tmpf{i}", [128, TT]) for i in range(4)]
        big = B("big", [128, 13312], BF16)
        big2 = B("big2", [128, 12288], BF16)
        NSLOT = 5
        slots = [B(f"slot{i}", [128, 4096], BF16) for i in range(NSLOT)]
        stg = [B(f"stg{i}", [128, 512]) for i in range(3)]
        PT = [B(f"PT{i}", [128, TT], BF16) for i in range(4)]
        tsc = [B(f"tsc{i}", [128, TT]) for i in range(3)]
        bq = B("bq", [128, 2, TT])
        oaT = B("oaT", [128, 4, TT], BF16); omT = B("omT", [128, 4, TT], BF16)
        coef = B("coef", [128, NL, NSQ, 9, 8])
        ngT = B("ngT", [128, NL, 48]); cwT = B("cwT", [128, NL, 32]); cbT = B("cbT", [128, NL, 8])
        nacol = B("nacol", [128, NL]); nmcol = B("nmcol", [128, NL]); nlam = B("nlam", [128, NL])
        bif = B("bif", [4, NL, 2]); nbf = B("nbf", [4, NL])
        CTn = B("CTn", [128, NL, 4, 129]); mstate = B("mstate", [4, NL])
        carry = B("carry", [128, NL, 8, 3]); kmax2 = B("kmax2", [128, NL, 8])
        CTb = [B(f"CTb{i}", [128, 4, 128], BF16) for i in range(2)]
        nbcb = [B(f"nbcb{i}", [128, 4, 128], BF16) for i in range(2)]
        g_negM = B("g_negM", [4, TT]); g_enm = B("g_enm", [4, TT])
        g_mst = B("g_mst", [4, 9]); g_Mend = B("g_Mend", [4, 8]); g_wc = B("g_wc", [4, 8])
        wcbc = B("wcbc", [128, 4, 8]); acol = B("acol", [64, 8, 4]); wscol = B("wscol", [64, 8, 4])
        small = B("small", [128, 8])
        ctok = B("ctok", [128, 512])
        ctokc = B("ctokc", [128, 128])
        scT = B("scT", [128, 8, NSQ], BF16)
        banks = [(k.psum(f"bank{i}", [128, 512], F32), Tr(f"bank{i}", excl=True)) for i in range(8)]
        bstate = {"i": 0, "avail": list(range(8))}

        def nb():
            a = bstate["avail"]
            bstate["i"] = (bstate["i"] + 1) % len(a)
            return banks[a[bstate["i"]]]

        class V:
            def __init__(self, ap, tr):
                self.t = ap
                self.T = tr
        gA = V(tsc[0].t, tsc[0].T); gB = V(tsc[1].t, tsc[1].T); g_t = V(tsc[2].t, tsc[2].T)
        g_a = V(bq.t[:, 0, :], bq.T); g_am = V(bq.t[:, 1, :], bq.T)
        g_sc = V(stg[0].t[0:4, :], stg[0].T); g_wi = V(stg[2].t[0:4, :], stg[2].T); g_ws = V(tmpf[3].t[0:4, :], tmpf[3].T)

        bigf = big.t[:, :].bitcast(F32)
        big2f = big2.t[:, :].bitcast(F32)
        groups = {"ffn": {}, "att": {}, "ml": {}, "io": {}}

        def view(group, name, ap):
            v = V(ap, Tr(name))
            groups[group][name] = v
            return v

        hid = view("ffn", "hid", big.t[:, 0:NFF * TT].rearrange("p (c t) -> p c t", c=NFF))
        pastK = [view("att", f"pastK{i}", big.t[:, i * 3584:(i + 1) * 3584]) for i in range(2)]
        kTc = view("att", "kTc", big.t[:, 7168:9216].rearrange("p (c t) -> p c t", c=4))
        vtok = view("att", "vtok", big.t[:, 9216:11264].rearrange("p (c t) -> p c t", c=4))
        pastV = [view("att", f"pastV{i}", big2.t[:, i * 3584:(i + 1) * 3584].rearrange("p (b e) -> p b e", e=128)) for i in range(2)]
        qpad = view("att", "qpad", big2.t[:, 7168:11264].rearrange("p (h c t) -> p h c t", h=4, c=2))
        qmT = view("ml", "qmT", big.t[:, 0:2048].rearrange("p (c t) -> p c t", c=4))
        kmT = view("ml", "kmT", big.t[:, 2048:4096].rearrange("p (c t) -> p c t", c=4))
        qw = view("ml", "qw", big.t[:, 4096:6144].rearrange("p (c t) -> p c t", c=4))
        ogT = view("ml", "ogT", big.t[:, 6144:8192].rearrange("p (c t) -> p c t", c=4))
        mvtok = view("ml", "mvtok", big.t[:, 8192:8192 + 8 * 4 * 129].rearrange("p (b h e) -> p b h e", b=8, h=4))
        kws = [view("ml", f"kws{i}", big.t[:, 12320 + i * 128:12320 + (i + 1) * 128]) for i in range(2)]
        swT = [view("ml", f"swT{i}", big.t[:, 12576 + i * 64:12576 + (i + 1) * 64]) for i in range(4)]
        ucat = view("ml", "ucat", big2f[:, 0:4 * (TT + 3)].rearrange("p (c t) -> p c t", c=4))
        kmf = view("ml", "kmf", big2f[:, 2064:2064 + 2048].rearrange("p (c t) -> p c t", c=4))
        negMbc = view("ml", "negMbc", big2f[:, 4112:4112 + 1024].rearrange("p (c t) -> p c t", c=2))
        ETf = [view("ml", f"ETf{i}", big2f[:, 5136 + i * 64:5136 + (i + 1) * 64]) for i in range(4)]
        xtok = view("io", "xtok", bigf[:, 0:4096].rearrange("p (b f) -> p b f", f=1024))
        modv = view("io", "modv", big2f[:, 0:72 * NSQ].rearrange("p (c s) -> p c s", s=NSQ))
        merged = sq

        def phase(g):
            evs = {}
            for og, vs in groups.items():
                if og == g:
                    continue
                for v in vs.values():
                    for s, val in v.T.r.items():
                        if evs.get(s, 0) < val:
                            evs[s] = val
                    if v.T.w is not None and evs.get(v.T.w[0], 0) < v.T.w[1]:
                        evs[v.T.w[0]] = v.T.w[1]
            for v in groups[g].values():
                for s, val in evs.items():
                    if v.T.r.get(s, 0) < val:
                        v.T.r[s] = val

        wconv_tr = [Tr(f"wconv{l}") for l in range(NL)]

        def slab(ap2d, nk, c0, n, l):
            return (ap2d[0:nk * 128, c0:c0 + n].rearrange("(c p) n -> p c n", p=128), nk, n, l)

        def plan_layer(l):
            out = []
            for i in range(2):
                ff = []
                for j in range(6):
                    n = 512 if j < 5 else 256
                    ff.append(slab(wb1[l, i], 8, j * 512, n, l))
                    ff.append(slab(wb3[l, i], 8, j * 512, n, l))
                for oc in range(8):
                    ff.append(slab(wb2[l, i], NFF, oc * 128, 128, l))
                out.append(ff)
            mx = []
            for c0 in (OFF_AQ, OFF_AK, OFF_AV, OFF_MQ, OFF_MK, OFF_MV, OFF_MO):
                mx.append(slab(wbin[l], 8, c0, 512, l))
            mx.append(slab(wbin[l], 8, OFF_MI, 8, l))
            mx.append(slab(wbpa[l], 4, 0, 1024, l))
            mx.append(slab(wbin[l], 8, OFF_G, 512, l)); mx.append(slab(wbin[l], 8, OFF_G + 512, 512, l))
            mx.append(slab(wbpb[l], 4, 0, 1024, l))
            mx.append(slab(wbin[l], 8, OFF_G + 1024, 512, l)); mx.append(slab(wbin[l], 8, OFF_G + 1536, 512, l))
            mx.append(slab(wbout[l], 8, 0, 512, l)); mx.append(slab(wbout[l], 8, 512, 512, l))
            return out[0] + mx + out[1]

        plan = []
        for _ in range(NPS * NTILE + NSS):
            for l in range(NL):
                plan.extend(plan_layer(l))
        ws_state = {"issued": 0, "used": 0}

        def ws_next():
            while ws_state["issued"] < min(len(plan), ws_state["used"] + NSLOT - 2):
                i = ws_state["issued"]
                ap3, nk, n, l = plan[i]
                s = slots[i % NSLOT]
                k.dma(SP, s.t[:, 0:nk * n].rearrange("p (c n) -> p c n", c=nk), ap3, R=[wconv_tr[l]], W=[s.T])
                ws_state["issued"] += 1
            i = ws_state["used"]
            ap3, nk, n, l = plan[i]
            s = slots[i % NSLOT]
            ws_state["used"] += 1
            return s.t[:, 0:nk * n].rearrange("p (c n) -> p c n", c=nk), s.T

        k.dma(SP, cst.t[:, :], consts[:, :], W=[cst.T])
        memset(DVE, onesb.t[:, :], 1.0, [onesb.T])
        memset(DVE, epsb.t[:, :], EPS, [epsb.T])
        cp(DVE, oneh.t[:, :, :], cst.t[:, C_OH:C_OH + 256].rearrange("p (c n) -> p c n", c=2), [cst.T], [oneh.T])
        memset(POOL, big.t[:, :], 0.0, [v.T for g in groups.values() for v in g.values()])
        memset(POOL, big2.t[:, :], 0.0, [v.T for g in groups.values() for v in g.values()])

        def conv_weights(l):
            def c2(dst, src, rows, step):
                for r0 in range(0, rows, step):
                    k.dma(POOL, dst[r0:r0 + step, :], src[r0:r0 + step, :], W=[wconv_tr[l]], prim=wconv_tr[l], nowaw=True)
            for i in range(2):
                c2(wb1[l, i], w_ffn1[l, i], D, 256); c2(wb3[l, i], w_ffn3[l, i], D, 256); c2(wb2[l, i], w_ffn2[l, i], DFF, 256)
            c2(wbin[l], w_in[l], D, 128); c2(wbpa[l], w_pa[l], 512, 256); c2(wbpb[l], w_pb[l], 512, 256); c2(wbout[l], w_out[l], D, 256)

        def load_T(src2d, rows):
            k.dma(SP, ctok.t[0:rows, 0:128], src2d, W=[ctok.T])
            bt, btr = nb()
            trp(bt[:, 0:rows], ctok.t[0:rows, 0:128], ident[0:rows, 0:rows], [ctok.T, cst.T], [btr])
            return bt, btr

        conv_weights(0)
        for l in range(NL):
            ngv = norm_g[l].rearrange("a (c p) -> (a c) p", p=128)
            bt, btr = load_T(ngv[0:32, :], 32)
            cp(DVE, ngT.t[:, l, 0:32], bt[:, 0:32], [btr], [ngT.T])
            bt, btr = load_T(ngv[32:48, :], 16)
            cp(DVE, ngT.t[:, l, 32:48], bt[:, 0:16], [btr], [ngT.T])
            bt, btr = load_T(conv_w[l].rearrange("a (c p) -> (a c) p", p=128), 32)
            cp(DVE, cwT.t[:, l, :], bt[:, 0:32], [btr], [cwT.T])
            bt, btr = load_T(conv_b[l].rearrange("(c p) -> c p", p=128), 8)
            cp(DVE, cbT.t[:, l, :], bt[:, 0:8], [btr], [cbT.T])
            k.dma(SP, nacol.t[:, l:l + 1], norm_a[l].rearrange("(p o) -> p o", o=1), W=[nacol.T])
            k.dma(SP, nmcol.t[:, l:l + 1], norm_m[l].rearrange("(p o) -> p o", o=1), W=[nmcol.T])
            k.dma(SP, bif.t[:, l, 0:1], b_if[l, 0:4].rearrange("(h o) -> h o", o=1), W=[bif.T])
            k.dma(SP, bif.t[:, l, 1:2], b_if[l, 4:8].rearrange("(h o) -> h o", o=1), W=[bif.T])
        for l in range(NL):
            ts(DVE, nacol.t[:, l:l + 1], nacol.t[:, l:l + 1], 1.0 - LAM_INIT[l], ALU.mult, [nacol.T], [nacol.T])
        ts(DVE, nbf.t[:, :], bif.t[:, :, 1], -1.0, ALU.mult, [bif.T], [nbf.T])
        for l in range(NL):
            k.dma(SP, ctok.t[0:1, 0:256], lam[l].rearrange("(o a) d -> o (a d)", o=1), W=[ctok.T])
            tt(DVE, ctok.t[0:1, 256:320], ctok.t[0:1, 0:64], ctok.t[0:1, 64:128], ALU.mult, [ctok.T], [ctok.T])
            tt(DVE, ctok.t[0:1, 320:384], ctok.t[0:1, 128:192], ctok.t[0:1, 192:256], ALU.mult, [ctok.T], [ctok.T])
            k.op(DVE, lambda e: e.tensor_reduce(out=ctok.t[0:1, 384:386], in_=ctok.t[0:1, 256:384].rearrange("p (a d) -> p a d", a=2), axis=AX.X, op=ALU.add), [ctok.T], [ctok.T])
            act(ctok.t[0:1, 386:388], ctok.t[0:1, 384:386], AF.Exp, [ctok.T], [ctok.T])
            tt(DVE, ctok.t[0:1, 388:389], ctok.t[0:1, 387:388], ctok.t[0:1, 386:387], ALU.subtract, [ctok.T], [ctok.T])
            ts(DVE, ctok.t[0:1, 389:390], ctok.t[0:1, 388:389], -LAM_INIT[l], ALU.add, [ctok.T], [ctok.T])
            bt, btr = nb()
            mm(bt[:, 0:1], cst.t[0:1, C_SEL:C_SEL + 128], ctok.t[0:1, 389:390], [cst.T, ctok.T], [btr])
            cp(DVE, nlam.t[:, l:l + 1], bt[:, 0:1], [btr], [nlam.T])

        bt, btr = load_T(cc.rearrange("s (c p) -> (s c) p", p=128), NSQ * 8)
        act(scT.t[:, :, :].rearrange("p c s -> p s c"), bt[:, 0:NSQ * 8].rearrange("p (s c) -> p s c", c=8), AF.Silu, [btr], [scT.T])
        phase("io")
        badT = tmpf[0]
        for l in range(NL):
            bt, btr = load_T(b_ada[l].rearrange("(c p) -> c p", p=128), 72)
            cp(DVE, badT.t[:, 0:72], bt[:, 0:72], [btr], [badT.T])
            for j in range(18):
                s = slots[j % NSLOT]
                k.dma(POOL, s.t[:, 0:4096].rearrange("p (c n) -> p c n", c=8), w_ada[l][:, j * 512:(j + 1) * 512].rearrange("(c p) n -> p c n", p=128), W=[s.T])
                for q4 in range(4):
                    ch = j * 4 + q4
                    bt, btr = nb()
                    for kc in range(8):
                        mm(bt[:, 0:NSQ], s.t[:, kc * 512 + q4 * 128: kc * 512 + (q4 + 1) * 128], scT.t[:, kc, :], [s.T, scT.T], [btr], start=(kc == 0), stop=(kc == 7))
                    ts(DVE, modv.t[:, ch, :], bt[:, 0:NSQ], badT.t[:, ch:ch + 1], ALU.add, [btr, badT.T], [modv.T])
            for sq_i in range(NSQ):
                for j in range(3):
                    sh = modv.t[:, (3 * j) * 8:(3 * j) * 8 + 8, sq_i]
                    scl = modv.t[:, (3 * j + 1) * 8:(3 * j + 1) * 8 + 8, sq_i]
                    gg = modv.t[:, (3 * j + 2) * 8:(3 * j + 2) * 8 + 8, sq_i]
                    stt(coef.t[:, l, sq_i, 3 * j, :], scl, 1.0, ngT.t[:, l, (2 * j) * 8:(2 * j) * 8 + 8], ALU.add, ALU.mult, [modv.T, ngT.T], [coef.T])
                    cp(DVE, coef.t[:, l, sq_i, 3 * j + 1, :], sh, [modv.T], [coef.T])
                    stt(coef.t[:, l, sq_i, 3 * j + 2, :], gg, (1.0 if j == 1 else 0.5), ngT.t[:, l, (2 * j + 1) * 8:(2 * j + 1) * 8 + 8], ALU.mult, ALU.mult, [modv.T, ngT.T], [coef.T])
            if l + 1 < NL:
                conv_weights(l + 1)

        def rms_bc(src, srcT, nchunk, T, dst, mean_scale):
            act(sq.t[:, 0:nchunk, 0:T], src, AF.Square, [srcT], [sq.T])
            bt, btr = nb()
            for c in range(nchunk):
                mm(bt[:, 0:T], onesb.t[:, :], sq.t[:, c, 0:T], [onesb.T, sq.T], [btr], start=(c == 0), stop=(c == nchunk - 1))
            act(dst.t[:, 0:T], bt[:, 0:T], AF.Sqrt, [btr, epsb.T], [dst.T], scale=mean_scale, bias=epsb.t[:, 0:1])
            recip(dst.t[:, 0:T], dst.t[:, 0:T], [dst.T], [dst.T])

        def prenorm(l, sqi, j, T):
            rms_bc(xT.t[:, :, 0:T], xT.T, 8, T, rbc, 1.0 / D)
            for c in range(8):
                tm = tmpf[c % 4]
                tt(DVE, tm.t[:, 0:T], xT.t[:, c, 0:T], rbc.t[:, 0:T], ALU.mult, [xT.T, rbc.T], [tm.T])
                act(hT.t[:, c, 0:T], tm.t[:, 0:T], AF.Identity, [tm.T, coef.T], [hT.T],
                    scale=coef.t[:, l, sqi, 3 * j, c:c + 1], bias=coef.t[:, l, sqi, 3 * j + 1, c:c + 1])

        def postnorm(l, sqi, j, T):
            rms_bc(yT.t[:, :, 0:T], yT.T, 8, T, rbc, 1.0 / D)
            for c in range(8):
                tm = tmpf[c % 4]
                tt(DVE, tm.t[:, 0:T], yT.t[:, c, 0:T], rbc.t[:, 0:T], ALU.mult, [yT.T, rbc.T], [tm.T])
                stt(xT.t[:, c, 0:T], tm.t[:, 0:T], coef.t[:, l, sqi, 3 * j + 2, c:c + 1], xT.t[:, c, 0:T], ALU.mult, ALU.add, [tm.T, coef.T, xT.T], [xT.T])

        def ffn(l, sqi, j, T):
            prenorm(l, sqi, j, T)
            tap(1, hT.t[:, :, :].rearrange("p c t -> p (c t)"), hT.T, 4096)
            tap(7, rbc.t[:, :], rbc.T, 512)
            phase("ffn")
            for sj in range(6):
                n = 512 if sj < 5 else 256
                w1, w1t = ws_next()
                w3, w3t = ws_next()
                tap(9, w1.rearrange("p c n -> p (c n)"), w1t, 4096)
                tap(10, w3.rearrange("p c n -> p (c n)"), w3t, 4096)
                for q4 in range(n // 128):
                    ch = sj * 4 + q4
                    ba, bat = nb()
                    bb, bbt = nb()
                    for kc in range(8):
                        mm(ba[:, 0:T], w1[:, kc, q4 * 128:(q4 + 1) * 128], hT.t[:, kc, 0:T], [w1t, hT.T], [bat], start=(kc == 0), stop=(kc == 7))
                    for kc in range(8):
                        mm(bb[:, 0:T], w3[:, kc, q4 * 128:(q4 + 1) * 128], hT.t[:, kc, 0:T], [w3t, hT.T], [bbt], start=(kc == 0), stop=(kc == 7))
                    tm = tmpf[ch % 4]
                    act(tm.t[:, 0:T], ba[:, 0:T], AF.Silu, [bat], [tm.T])
                    tt(DVE, hid.t[:, ch, 0:T], tm.t[:, 0:T], bb[:, 0:T], ALU.mult, [tm.T, bbt], [hid.T])
            for oc in range(8):
                w2, w2t = ws_next()
                bo, bot = nb()
                for kc in range(NFF):
                    mm(bo[:, 0:T], w2[:, kc, :], hid.t[:, kc, 0:T], [w2t, hid.T], [bot], start=(kc == 0), stop=(kc == NFF - 1))
                act(yT.t[:, oc, 0:T], bo[:, 0:T], AF.Copy, [bot], [yT.T])
            tap(2, yT.t[:, :, :].rearrange("p c t -> p (c t)"), yT.T, 4096)
            tap(8, hid.t[:, 0:8, :].rearrange("p c t -> p (c t)"), hid.T, 4096)
            postnorm(l, sqi, j, T)

        kv_tr = [[Tr(f"kv{s}_{l}") for l in range(NL)] for s in range(max(NPS, 1))]
        PTf = [B(f"PTf{i}", [128, 16]) for i in range(4)]
        kst = V(bq.t[:, :, :].rearrange("p a b -> p (a b)").rearrange("p (b e) -> p b e", e=128), bq.T)

        def mixer(l, sqi, T, is_prompt, seq, tj):
            L = 64 if is_prompt else 16
            MB = T // L
            NB = max(1, T // 128)
            tb = min(T, 128)
            last_tile = (not is_prompt) or (tj == NTILE - 1)
            prenorm(l, sqi, 1, T)
            tap(4, hT.t[:, :, :].rearrange("p c t -> p (c t)"), hT.T, 4096)
            tap(6, rbc.t[:, :], rbc.T, 512)
            phase("att")
            memset(POOL, qpad.t[64:128, :, 0, :], 0.0, [qpad.T])
            memset(POOL, qpad.t[0:64, :, 1, :], 0.0, [qpad.T])
            stop_at(21)
            wq, wqt = ws_next()
            for h in range(4):
                bt, btr = nb()
                for kc in range(8):
                    mm(bt[:, 0:T], wq[:, kc, h * 128:(h + 1) * 128], hT.t[:, kc, 0:T], [wqt, hT.T], [btr], start=(kc == 0), stop=(kc == 7))
                act(qpad.t[0:64, h, 0, 0:T], bt[0:64, 0:T], AF.Copy, [btr], [qpad.T], scale=0.125)
                act(qpad.t[64:128, h, 1, 0:T], bt[64:128, 0:T], AF.Copy, [btr], [qpad.T], scale=0.125)
            stop_at(22)
            wk, wkt = ws_next()
            for h in range(4):
                bt, btr = nb()
                for kc in range(8):
                    mm(bt[:, 0:T], wk[:, kc, h * 128:(h + 1) * 128], hT.t[:, kc, 0:T], [wkt, hT.T], [btr], start=(kc == 0), stop=(kc == 7))
                act(kTc.t[:, h, 0:T], bt[:, 0:T], AF.Copy, [btr], [kTc.T])
            kout = pk[l, seq] if is_prompt else sk[l, seq]
            vout = pv[l, seq] if is_prompt else sv[l, seq]
            for b in range(NB):
                bt, btr = nb()
                for kc in range(8):
                    mm(bt[0:tb, :], hT.t[:, kc, b * 128:b * 128 + tb], wk[:, kc, :], [hT.T, wkt], [btr], start=(kc == 0), stop=(kc == 7))
                st = stg[b % 3]
                cp(DVE, st.t[0:tb, :], bt[0:tb, :], [btr], [st.T])
                k.dma(POOL, kout[tj * 512 + b * 128: tj * 512 + b * 128 + tb, :], st.t[0:tb, :], R=[st.T], prim=st.T, is_output=True)
            stop_at(23)
            wv, wvt = ws_next()
            for b in range(NB):
                bt, btr = nb()
                for kc in range(8):
                    mm(bt[0:tb, :], hT.t[:, kc, b * 128:b * 128 + tb], wv[:, kc, :], [hT.T, wvt], [btr], start=(kc == 0), stop=(kc == 7))
                st = stg[(b + 1) % 3]
                cp(DVE, st.t[0:tb, :], bt[0:tb, :], [btr], [st.T])
                if cfg.get("var", 1) != 1:
                    act(vtok.t[0:tb, b, :], bt[0:tb, :], AF.Copy, [btr], [vtok.T])
                else:
                    cp(DVE, vtok.t[0:tb, b, :], st.t[0:tb, :], [st.T], [vtok.T])
                k.dma(POOL, vout[tj * 512 + b * 128: tj * 512 + b * 128 + tb, :], st.t[0:tb, :], R=[st.T], prim=st.T, is_output=True)
            if is_prompt and tj < NTILE - 1:
                for h in range(4):
                    k.dma(POOL, kscr[seq, l, h][:, tj * 512:(tj + 1) * 512], kTc.t[:, h, :], R=[kTc.T], W=[kv_tr[seq][l]], prim=kTc.T)
                    k.dma(POOL, vscr[seq, l, h][:, tj * 4:(tj + 1) * 4, :], vtok.t[:, :, h * 128:(h + 1) * 128], R=[vtok.T], W=[kv_tr[seq][l]], prim=vtok.T)
            stop_at(3)
            if tj == 0:
                memset(DVE, kmax2.t[:, l, :], 0.0, [kmax2.T])
            npast = tj * 4 if is_prompt else PAST // 128
            rbase = npast
            for h in range(4):
                par = h % 2
                pK, pV = pastK[par], pastV[par]
                if is_prompt:
                    if npast > 0:
                        k.dma(POOL, pK.t[:, 0:npast * 128], kscr[seq, l, h][:, 0:npast * 128], R=[kv_tr[seq][l]], W=[pK.T])
                        k.dma(POOL, pV.t[:, 0:npast, :], vscr[seq, l, h][:, 0:npast, :], R=[kv_tr[seq][l]], W=[pV.T])
                else:
                    k.dma(POOL, pV.t[:, 0:npast, :], cache_v[l, seq][:, h * 128:(h + 1) * 128].rearrange("(b p) e -> p b e", p=128), W=[pV.T])
                    for half in range(2):
                        k.dma(SP, kst.t[:, :, :], cache_k[l, seq][half * 1024:(half + 1) * 1024, h * 128:(h + 1) * 128].rearrange("(b p) e -> p b e", p=128), W=[kst.T])
                        for b4 in range(2):
                            bt, btr = nb()
                            for b in range(4):
                                trp(bt[:, b * 128:(b + 1) * 128], kst.t[:, b4 * 4 + b, :], ident, [kst.T, cst.T], [btr])
                            c0 = (half * 8 + b4 * 4) * 128
                            act(pK.t[:, c0:c0 + 512], bt[:, :], AF.Copy, [btr], [pK.T])
                ksrcs = [(kTc.t[:, h, 0:T], kTc.T, T)]
                if not is_prompt:
                    ksrcs += [(pK.t[:, g * 512:(g + 1) * 512], pK.T, 512) for g in range(npast // 4)]
                for gi_, (ksrc, ktr, n_) in enumerate(ksrcs):
                    act(sq.t[:, 2 + gi_ % 2, 0:n_], ksrc, AF.Square, [ktr], [sq.T])
                    for c in range(2):
                        bt, btr = nb()
                        mm(bt[:, 0:n_], oneh.t[:, c, :], sq.t[:, 2 + gi_ % 2, 0:n_], [oneh.T, sq.T], [btr])
                        rmax(small.t[:, c:c + 1], bt[:, 0:n_], [btr], [small.T])
                        tt(DVE, kmax2.t[:, l, h * 2 + c:h * 2 + c + 1], kmax2.t[:, l, h * 2 + c:h * 2 + c + 1], small.t[:, c:c + 1], ALU.max, [small.T, kmax2.T], [kmax2.T])
                for c in range(2):
                    act(sq.t[:, c, 0:T], qpad.t[:, h, c, 0:T], AF.Square, [qpad.T], [sq.T])
                    bt, btr = nb()
                    mm(bt[:, 0:T], onesb.t[:, :], sq.t[:, c, 0:T], [onesb.T, sq.T], [btr])
                    tm = tmpf[c]
                    act(tm.t[:, 0:T], bt[:, 0:T], AF.Sqrt, [btr, kmax2.T], [tm.T], scale=kmax2.t[:, l, h * 2 + c:h * 2 + c + 1])
                    stt(bq.t[:, c, 0:T], cst.t[:, C_QP:C_QP + T], -SLOPES[h], tm.t[:, 0:T], ALU.mult, ALU.subtract, [cst.T, tm.T], [bq.T])
                acc = [banks[i] for i in (0, 1, 2, 3)]
                bstate["avail"] = [4, 5, 6, 7]
                nblk = npast + NB
                steps = [(kb, c) for kb in range(nblk) for c in range(2)]
                LA = 3

                def geom(kb):
                    diag = kb >= npast
                    d = kb - npast
                    q0 = d * 128 if diag else 0
                    nk = tb if diag else 128
                    return diag, d, q0, nk

                def emit_S(i):
                    kb, c = steps[i]
                    diag, d, q0, nk = geom(kb)
                    sb_, sbt = banks[4 + i % 4]
                    if diag:
                        mm(sb_[0:nk, q0:T], kTc.t[:, h, d * 128:d * 128 + nk], qpad.t[:, h, c, q0:T], [kTc.T, qpad.T], [sbt])
                    else:
                        mm(sb_[0:nk, q0:T], pK.t[:, kb * 128:(kb + 1) * 128], qpad.t[:, h, c, q0:T], [pK.T, qpad.T], [sbt])

                def emit_soft(i):
                    kb, c = steps[i]
                    diag, d, q0, nk = geom(kb)
                    sb_, sbt = banks[4 + i % 4]
                    tsb = tsc[i % 3]
                    if diag:
                        stt(tsb.t[0:nk, q0:T], cst.t[0:nk, C_FP:C_FP + T - q0], SLOPES[h], sb_[0:nk, q0:T], ALU.mult, ALU.add, [cst.T, sbt], [tsb.T])
                        tt(DVE, tsb.t[0:nk, q0:T], tsb.t[0:nk, q0:T], bq.t[0:nk, c, q0:T], ALU.add, [tsb.T, bq.T], [tsb.T])
                        bcol = cst.t[0:nk, C_BK + h * 36 + 32 + d:C_BK + h * 36 + 33 + d]
                    else:
                        tt(DVE, tsb.t[0:nk, q0:T], sb_[0:nk, q0:T], bq.t[0:nk, c, q0:T], ALU.add, [sbt, bq.T], [tsb.T])
                        r = rbase - kb
                        bcol = cst.t[0:nk, C_BK + h * 36 + r:C_BK + h * 36 + r + 1]
                    p = PT[i % 4]
                    act(p.t[0:nk, q0:T], tsb.t[0:nk, q0:T], AF.Exp, [tsb.T, cst.T], [p.T], bias=bcol)

                def emit_PV(i):
                    kb, c = steps[i]
                    diag, d, q0, nk = geom(kb)
                    p = PT[i % 4]
                    first = (kb == 0)
                    last = (kb == nblk - 1)
                    if diag and not is_prompt:
                        mm(acc[c][0][:, T:2 * T], vtok.t[0:nk, d, h * 128:(h + 1) * 128], p.t[0:nk, 0:T], [vtok.T, p.T], [acc[c][1]], start=True, stop=True, sg=True)
                        mm(acc[2 + c][0][:, T:2 * T], onesb.t[0:nk, :], p.t[0:nk, 0:T], [onesb.T, p.T], [acc[2 + c][1]], start=True, stop=True, sg=True)
                        return
                    if not is_prompt:
                        last = (kb == npast - 1)
                    if diag:
                        mm(acc[c][0][:, q0:T], vtok.t[0:nk, d, h * 128:(h + 1) * 128], p.t[0:nk, q0:T], [vtok.T, p.T], [acc[c][1]], start=first, stop=last, sg=True)
                    else:
                        mm(acc[c][0][:, q0:T], pV.t[:, kb, :], p.t[0:nk, q0:T], [pV.T, p.T], [acc[c][1]], start=first, stop=last, sg=True)
                    mm(acc[2 + c][0][:, q0:T], onesb.t[0:nk, :], p.t[0:nk, q0:T], [onesb.T, p.T], [acc[2 + c][1]], start=first, stop=last, sg=True)

                for i in range(min(LA, len(steps))):
                    emit_S(i)
                for i in range(len(steps)):
                    emit_soft(i)
                    if i + LA < len(steps):
                        emit_S(i + LA)
                    emit_PV(i)
                bstate["avail"] = list(range(8))
                r0, r1, o0 = tmpf[0], tmpf[1], tmpf[2]
                if is_prompt:
                    srcs = [acc[i][0][:, 0:T] for i in range(4)]
                    srct = [acc[i][1] for i in range(4)]
                else:
                    srcs, srct = [], []
                    for i in range(4):
                        cp(DVE, tsc[i % 3].t[:, 0:T], acc[i][0][:, 0:T], [acc[i][1]], [tsc[i % 3].T])
                        dsti = PTf[i]
                        tt(DVE, dsti.t[:, 0:T], tsc[i % 3].t[:, 0:T], acc[i][0][:, T:2 * T], ALU.add, [tsc[i % 3].T, acc[i][1]], [dsti.T])
                        srcs.append(dsti.t[:, 0:T]); srct.append(dsti.T)
                recip(r0.t[:, 0:T], srcs[2], [srct[2]], [r0.T])
                recip(r1.t[:, 0:T], srcs[3], [srct[3]], [r1.T])
                tt(DVE, o0.t[:, 0:T], srcs[0], r0.t[:, 0:T], ALU.mult, [srct[0], r0.T], [o0.T])
                tt(DVE, r1.t[:, 0:T], srcs[1], r1.t[:, 0:T], ALU.mult, [srct[1], r1.T], [r1.T])
                stt(o0.t[:, 0:T], r1.t[:, 0:T], nlam.t[:, l:l + 1], o0.t[:, 0:T], ALU.mult, ALU.add, [r1.T, nlam.T, o0.T], [o0.T])
                rms_bc(o0.t[:, 0:T].rearrange("p (c t) -> p c t", c=1), o0.T, 1, T, r0, 1.0 / 128)
                stt(oaT.t[:, h, 0:T], o0.t[:, 0:T], nacol.t[:, l:l + 1], r0.t[:, 0:T], ALU.mult, ALU.mult, [o0.T, nacol.T, r0.T], [oaT.T])

            stop_at(4)
            phase("ml")
            if is_prompt:
                if tj == 0:
                    memset(DVE, carry.t[:, l, :, :], 0.0, [carry.T])
            else:
                for half in range(2):
                    k.dma(SP, ctok.t[0:3, :], state_conv[l, seq][:, half * 512:(half + 1) * 512], W=[ctok.T])
                    bt, btr = nb()
                    for c4 in range(4):
                        trp(bt[:, c4 * 3:(c4 + 1) * 3], ctok.t[0:3, c4 * 128:(c4 + 1) * 128], ident[0:3, 0:3], [ctok.T, cst.T], [btr])
                    cp(DVE, carry.t[:, l, half * 4:half * 4 + 4, :], bt[:, 0:12].rearrange("p (c j) -> p c j", j=3), [btr], [carry.T])
            for half in range(2):
                wmq, wmqt = ws_next()
                cp(DVE, ucat.t[:, :, 0:3], carry.t[:, l, half * 4:half * 4 + 4, :], [carry.T], [ucat.T])
                for h in range(4):
                    bt, btr = nb()
                    for kc in range(8):
                        mm(bt[:, 0:T], wmq[:, kc, h * 128:(h + 1) * 128], hT.t[:, kc, 0:T], [wmqt, hT.T], [btr], start=(kc == 0), stop=(kc == 7))
                    act(ucat.t[:, h, 3:3 + T], bt[:, 0:T], AF.Copy, [btr], [ucat.T])
                for h in range(4):
                    cch = half * 4 + h
                    tm = tmpf[h]
                    act(tm.t[:, 0:T], ucat.t[:, h, 3:3 + T], AF.Identity, [ucat.T, cwT.T, cbT.T], [tm.T],
                        scale=cwT.t[:, l, 3 * 8 + cch:3 * 8 + cch + 1], bias=cbT.t[:, l, cch:cch + 1])
                    for jj in range(3):
                        stt(tm.t[:, 0:T], ucat.t[:, h, jj:jj + T], cwT.t[:, l, jj * 8 + cch:jj * 8 + cch + 1], tm.t[:, 0:T], ALU.mult, ALU.add, [ucat.T, cwT.T, tm.T], [tm.T])
                    if half == 0:
                        act(qmT.t[:, h, 0:T], tm.t[:, 0:T], AF.Silu, [tm.T], [qmT.T])
                    else:
                        act(kmf.t[:, h, 0:T], tm.t[:, 0:T], AF.Silu, [tm.T], [kmf.T])
                        ts(DVE, kmf.t[:, h, 0:T], kmf.t[:, h, 0:T], 128.0 ** -0.5, ALU.mult, [kmf.T], [kmf.T])
                        cp(DVE, kmT.t[:, h, 0:T], kmf.t[:, h, 0:T], [kmf.T], [kmT.T])
                cp(DVE, carry.t[:, l, half * 4:half * 4 + 4, :], ucat.t[:, :, T:T + 3], [ucat.T], [carry.T])
                if last_tile:
                    cvo = pconv[l, seq] if is_prompt else sconv[l, seq]
                    bt, btr = nb()
                    for h in range(4):
                        mm(bt[0:3, h * 128:(h + 1) * 128], ucat.t[:, h, T:T + 3], ident, [ucat.T, cst.T], [btr])
                    st = stg[half]
                    cp(DVE, st.t[0:3, :], bt[0:3, :], [btr], [st.T])
                    k.dma(POOL, cvo[:, half * 512:(half + 1) * 512], st.t[0:3, :], R=[st.T], prim=st.T, is_output=True)
            wmv, wmvt = ws_next()
            memset(POOL, mvtok.t[:, :, :, :].rearrange("p b h e -> p (b h e)"), 0.0, [mvtok.T])
            for sw_ in swT:
                memset(POOL, sw_.t[:, :], 0.0, [sw_.T])
            for c in range(MB):
                bt, btr = nb()
                for kc in range(8):
                    mm(bt[0:L, :], hT.t[:, kc, c * L:(c + 1) * L], wmv[:, kc, :], [hT.T, wmvt], [btr], start=(kc == 0), stop=(kc == 7))
                act(mvtok.t[0:L, c, :, 0:128], bt[0:L, :].rearrange("p (h e) -> p h e", h=4), AF.Copy, [btr], [mvtok.T])
            for c in range(MB):
                memset(DVE, mvtok.t[0:L, c, :, 128:129], 1.0, [mvtok.T])

            wmo, wmot = ws_next()
            for h in range(4):
                bt, btr = nb()
                for kc in range(8):
                    mm(bt[:, 0:T], wmo[:, kc, h * 128:(h + 1) * 128], hT.t[:, kc, 0:T], [wmot, hT.T], [btr], start=(kc == 0), stop=(kc == 7))
                act(ogT.t[:, h, 0:T], bt[:, 0:T], AF.Sigmoid, [btr], [ogT.T])
            wg, wgt = ws_next()
            gi, git = nb()
            for kc in range(8):
                mm(gi[0:4, 0:T], wg[:, kc, 0:4], hT.t[:, kc, 0:T], [wgt, hT.T], [git], start=(kc == 0), stop=(kc == 7))
            gf, gft = nb()
            for kc in range(8):
                mm(gf[0:4, 0:T], wg[:, kc, 4:8], hT.t[:, kc, 0:T], [wgt, hT.T], [gft], start=(kc == 0), stop=(kc == 7))

            stop_at(5)
            def v3(bf, lo=0, hi=None):
                hi = L if hi is None else hi
                return bf.t[0:4, 0:T].rearrange("p (c t) -> p c t", t=L)[:, :, lo:hi]

            def scan(a, b, op):
                sh = 1
                while sh < L:
                    tt(DVE, v3(b, sh, L), v3(a, sh, L), v3(a, 0, L - sh), op, [a.T], [b.T])
                    cp(DVE, v3(b, 0, sh), v3(a, 0, sh), [a.T], [b.T])
                    a, b = b, a
                    sh *= 2
                return a

            act(gA.t[0:4, 0:T], gf[0:4, 0:T], AF.Exp, [gft, nbf.T], [gA.T], scale=-1.0, bias=nbf.t[:, l:l + 1])
            act(gA.t[0:4, 0:T], gA.t[0:4, 0:T], AF.Ln, [gA.T], [gA.T], bias=1.0)
            res = scan(gA, gB, ALU.add)
            cp(DVE, g_sc.t[:, 0:T], res.t[0:4, 0:T], [res.T], [g_sc.T])
            stt(g_a.t[0:4, 0:T], gi[0:4, 0:T], bif.t[:, l, 0:1], g_sc.t[:, 0:T], ALU.add, ALU.add, [git, bif.T, g_sc.T], [g_a.T])
            cp(DVE, gA.t[0:4, 0:T], g_a.t[0:4, 0:T], [g_a.T], [gA.T])
            res = scan(gA, gB, ALU.max)
            cp(DVE, g_am.t[0:4, 0:T], res.t[0:4, 0:T], [res.T], [g_am.T])
            if is_prompt:
                if tj == 0:
                    memset(DVE, mstate.t[:, l:l + 1], 0.0, [mstate.T])
            else:
                k.dma(SP, mstate.t[:, l:l + 1], state_m[l, seq].rearrange("(h o) -> h o", o=1), W=[mstate.T])
            cp(DVE, g_mst.t[:, 0:1], mstate.t[:, l:l + 1], [mstate.T], [g_mst.T])
            am_end = v3(g_am, L - 1, L)
            sc_end = v3(g_sc, L - 1, L)
            for c in range(MB):
                stt(g_mst.t[:, c + 1:c + 2], g_mst.t[:, c:c + 1], am_end[:, c, :], sc_end[:, c, :], ALU.max, ALU.subtract, [g_mst.T, g_am.T, g_sc.T], [g_mst.T])
            cp(DVE, mstate.t[:, l:l + 1], g_mst.t[:, MB:MB + 1], [g_mst.T], [mstate.T])
            tt(DVE, g_Mend.t[:, 0:MB].unsqueeze(2), g_mst.t[:, 0:MB].unsqueeze(2), am_end, ALU.max, [g_mst.T, g_am.T], [g_Mend.T])
            mst_b = g_mst.t[:, 0:MB].unsqueeze(2).broadcast_to([4, MB, L])
            mend_b = g_Mend.t[:, 0:MB].unsqueeze(2).broadcast_to([4, MB, L])
            tt(DVE, v3(g_t), v3(g_am), mst_b, ALU.max, [g_am.T, g_mst.T], [g_t.T])
            ts(DVE, g_negM.t[:, 0:T], g_t.t[0:4, 0:T], -1.0, ALU.mult, [g_t.T], [g_negM.T])
            tt(DVE, v3(g_t), v3(g_negM), mst_b, ALU.add, [g_negM.T, g_mst.T], [g_t.T])
            act(g_wi.t[:, 0:T], g_t.t[0:4, 0:T], AF.Exp, [g_t.T], [g_wi.T])
            tt(DVE, g_t.t[0:4, 0:T], g_sc.t[:, 0:T], g_negM.t[:, 0:T], ALU.add, [g_sc.T, g_negM.T], [g_t.T])
            act(g_enm.t[:, 0:T], g_t.t[0:4, 0:T], AF.Exp, [g_t.T], [g_enm.T])
            tt(DVE, v3(g_t), v3(g_a), mend_b, ALU.subtract, [g_a.T, g_Mend.T], [g_t.T])
            act(g_ws.t[:, 0:T], g_t.t[0:4, 0:T], AF.Exp, [g_t.T], [g_ws.T])
            tt(DVE, g_wc.t[:, 0:MB], g_mst.t[:, 0:MB], g_Mend.t[:, 0:MB], ALU.subtract, [g_mst.T, g_Mend.T], [g_wc.T])
            act(g_wc.t[:, 0:MB], g_wc.t[:, 0:MB], AF.Exp, [g_wc.T], [g_wc.T])
            if last_tile:
                mo_ = pm[l, seq] if is_prompt else sm[l, seq]
                k.dma(POOL, mo_.rearrange("(h o) -> h o", o=1), mstate.t[:, l:l + 1], R=[mstate.T], prim=mstate.T, is_output=True)
            bta, btar = nb()
            btw, btwr = nb()
            for c in range(MB):
                trp(bta[0:L, c * 4:(c + 1) * 4], g_a.t[0:4, c * L:(c + 1) * L], ident[0:4, 0:4], [g_a.T, cst.T], [btar])
                trp(btw[0:L, c * 4:(c + 1) * 4], g_ws.t[0:4, c * L:(c + 1) * L], ident[0:4, 0:4], [g_ws.T, cst.T], [btwr])
            cp(DVE, acol.t[0:L, 0:MB, :], bta[0:L, 0:MB * 4].rearrange("p (c h) -> p c h", h=4), [btar], [acol.T])
            cp(DVE, wscol.t[0:L, 0:MB, :], btw[0:L, 0:MB * 4].rearrange("p (c h) -> p c h", h=4), [btwr], [wscol.T])
            for h in range(4):
                selh = cst.t[0:4, C_SEL + h * 128:C_SEL + (h + 1) * 128]
                bt, btr = nb()
                mm(bt[:, 0:T], selh, g_wi.t[:, 0:T], [cst.T, g_wi.T], [btr])
                tt(DVE, qw.t[:, h, 0:T], qmT.t[:, h, 0:T], bt[:, 0:T], ALU.mult, [qmT.T, btr], [qw.T])
                bt, btr = nb()
                mm(bt[:, 0:MB], selh, g_wc.t[:, 0:MB], [cst.T, g_wc.T], [btr])
                cp(DVE, wcbc.t[:, h, 0:MB], bt[:, 0:MB], [btr], [wcbc.T])
            stop_at(6)
            if is_prompt:
                if tj == 0:
                    memset(DVE, CTn.t[:, l, :, :], 0.0, [CTn.T])
            else:
                for h in range(4):
                    k.dma(SP, ctokc.t[:, :], state_c[l, seq, h], W=[ctokc.T])
                    bt, btr = nb()
                    trp(bt[:, 0:128], ctokc.t[:, :], ident, [ctokc.T, cst.T], [btr])
                    cp(DVE, CTn.t[:, l, h, 0:128], bt[:, 0:128], [btr], [CTn.T])
                k.dma(SP, ctok.t[0:4, 0:128], state_n[l, seq], W=[ctok.T])
                bt, btr = nb()
                trp(bt[:, 0:4], ctok.t[0:4, 0:128], ident[0:4, 0:4], [ctok.T, cst.T], [btr])
                cp(DVE, CTn.t[:, l, :, 128:129], bt[:, 0:4].unsqueeze(2), [btr], [CTn.T])
            for hp in range(2):
                heads = (2 * hp, 2 * hp + 1)
                numb = {heads[0]: banks[0], heads[1]: banks[1]}
                denb = {heads[0]: banks[2], heads[1]: banks[3]}
                bstate["avail"] = [4, 5, 6, 7]
                for hi_, h in enumerate(heads):
                    selh = cst.t[0:4, C_SEL + h * 128:C_SEL + (h + 1) * 128]
                    bt, btr = nb()
                    mm(bt[:, 0:T], selh, g_negM.t[:, 0:T], [cst.T, g_negM.T], [btr])
                    tt(DVE, negMbc.t[0:L, hi_, 0:T].rearrange("p (c t) -> p c t", t=L), bt[0:L, 0:T].rearrange("p (c t) -> p c t", t=L),
                       cst.t[0:L, C_MN:C_MN + L].unsqueeze(1).broadcast_to([L, MB, L]), ALU.add, [btr, cst.T], [negMbc.T])
                msteps = [(c, hi_, h) for c in range(MB) for hi_, h in enumerate(heads)]

                def stage1(i):
                    c, hi_, h = msteps[i]
                    cs = slice(c * L, (c + 1) * L)
                    i4 = i % 4
                    sb_, sbt = nb()
                    mm(sb_[0:L, 0:L], kmT.t[:, h, cs], qmT.t[:, h, cs], [kmT.T, qmT.T], [sbt])
                    kt_, ktt = nb()
                    trp(kt_[0:L, 0:128], kmf.t[:, h, cs], ident, [kmf.T, cst.T], [ktt])
                    et = ETf[i4]
                    act(et.t[0:L, 0:L], negMbc.t[0:L, hi_, cs], AF.Exp, [negMbc.T, acol.T], [et.T], bias=acol.t[0:L, c, h:h + 1])
                    sw = swT[i4]
                    tt(DVE, sw.t[0:L, 0:L], sb_[0:L, 0:L], et.t[0:L, 0:L], ALU.mult, [sbt, et.T], [sw.T])
                    kw_ = kws[i % 2]
                    ts(DVE, kw_.t[0:L, :], kt_[0:L, 0:128], wscol.t[0:L, c, h:h + 1], ALU.mult, [ktt, wscol.T], [kw_.T])

                def stage2(i):
                    c, hi_, h = msteps[i]
                    cs = slice(c * L, (c + 1) * L)
                    par = c % 2
                    sw = swT[i % 4]
                    kw_ = kws[i % 2]
                    act(CTb[par].t[:, h, :], CTn.t[:, l, h, 0:128], AF.Copy, [CTn.T], [CTb[par].T])
                    cp(DVE, nbcb[par].t[:, h, :], CTn.t[:, l, h, 128:129].broadcast_to([128, 128]), [CTn.T], [nbcb[par].T])
                    ub, ubt = nb()
                    mm(ub[:, 0:129], kw_.t[0:L, :], mvtok.t[0:L, c, h, :], [kw_.T, mvtok.T], [ubt])
                    nbk, nbt = numb[h]
                    mm(nbk[:, cs], mvtok.t[:, c, h, 0:128], sw.t[:, 0:L], [mvtok.T, sw.T], [nbt], start=True, stop=False, sg=True)
                    dk, dt_ = denb[h]
                    mm(dk[:, cs], onesb.t[:, :], sw.t[:, 0:L], [onesb.T, sw.T], [dt_], start=True, stop=False, sg=True)
                    mm(nbk[:, cs], CTb[par].t[:, h, :], qw.t[:, h, cs], [CTb[par].T, qw.T], [nbt], start=False, stop=True, sg=True)
                    mm(dk[:, cs], nbcb[par].t[:, h, :], qw.t[:, h, cs], [nbcb[par].T, qw.T], [dt_], start=False, stop=True, sg=True)
                    stt(CTn.t[:, l, h, :], CTn.t[:, l, h, :], wcbc.t[:, h, c:c + 1], ub[:, 0:129], ALU.mult, ALU.add, [CTn.T, wcbc.T, ubt], [CTn.T])

                stage1(0)
                for i in range(len(msteps)):
                    if i + 1 < len(msteps):
                        stage1(i + 1)
                    stage2(i)
                for h in heads:
                    selh = cst.t[0:4, C_SEL + h * 128:C_SEL + (h + 1) * 128]
                    bt, btr = nb()
                    mm(bt[:, 0:T], selh, g_enm.t[:, 0:T], [cst.T, g_enm.T], [btr])
                    e_ = tmpf[0]
                    cp(DVE, e_.t[:, 0:T], bt[:, 0:T], [btr], [e_.T])
                    dk, dt_ = denb[h]
                    d_ = tmpf[1]
                    act(d_.t[:, 0:T], dk[:, 0:T], AF.Abs, [dt_], [d_.T])
                    tt(DVE, d_.t[:, 0:T], d_.t[:, 0:T], e_.t[:, 0:T], ALU.max, [d_.T, e_.T], [d_.T])
                    recip(d_.t[:, 0:T], d_.t[:, 0:T], [d_.T], [d_.T])
                    nbk, nbt = numb[h]
                    hm = tmpf[2]
                    tt(DVE, hm.t[:, 0:T], nbk[:, 0:T], d_.t[:, 0:T], ALU.mult, [nbt, d_.T], [hm.T])
                    tt(DVE, hm.t[:, 0:T], hm.t[:, 0:T], ogT.t[:, h, 0:T], ALU.mult, [hm.T, ogT.T], [hm.T])
                    rms_bc(hm.t[:, 0:T].rearrange("p (c t) -> p c t", c=1), hm.T, 1, T, e_, 1.0 / 128)
                    stt(omT.t[:, h, 0:T], hm.t[:, 0:T], nmcol.t[:, l:l + 1], e_.t[:, 0:T], ALU.mult, ALU.mult, [hm.T, nmcol.T, e_.T], [omT.T])
            bstate["avail"] = list(range(8))
            if last_tile:
                co = pc[l, seq] if is_prompt else sc[l, seq]
                no = pn[l, seq] if is_prompt else sn[l, seq]
                for h in range(4):
                    bt, btr = nb()
                    trp(bt[:, 0:128], CTn.t[:, l, h, 0:128], ident, [CTn.T, cst.T], [btr])
                    st = stg[h % 3]
                    cp(DVE, st.t[:, 0:128], bt[:, 0:128], [btr], [st.T])
                    k.dma(POOL, co[h], st.t[:, 0:128], R=[st.T], prim=st.T, is_output=True)
                bt, btr = nb()
                for h in range(4):
                    mm(bt[0:1, h * 128:(h + 1) * 128], CTn.t[:, l, h, 128:129], ident, [CTn.T, cst.T], [btr])
                st = stg[1]
                cp(DVE, st.t[0:1, :], bt[0:1, :], [btr], [st.T])
                k.dma(POOL, no.rearrange("(o h) d -> o (h d)", o=1), st.t[0:1, :], R=[st.T], prim=st.T, is_output=True)
            stop_at(7)
            for ph in range(2):
                src = oaT if ph == 0 else omT
                wp, wpt = ws_next()
                g0 = ws_next()
                g1 = ws_next()
                for oc in range(8):
                    gsl, gslt = (g0 if oc < 4 else g1)
                    bg, bgt = nb()
                    for kc in range(8):
                        mm(bg[:, 0:T], gsl[:, kc, (oc % 4) * 128:(oc % 4 + 1) * 128], hT.t[:, kc, 0:T], [gslt, hT.T], [bgt], start=(kc == 0), stop=(kc == 7))
                    bp, bpt = nb()
                    for kc in range(4):
                        mm(bp[:, 0:T], wp[:, kc, oc * 128:(oc + 1) * 128], src.t[:, kc, 0:T], [wpt, src.T], [bpt], start=(kc == 0), stop=(kc == 3))
                    tm = tmpf[oc % 4]
                    act(tm.t[:, 0:T], bg[:, 0:T], AF.Sigmoid, [bgt], [tm.T])
                    if ph == 0:
                        tt(DVE, merged.t[:, oc, 0:T], tm.t[:, 0:T], bp[:, 0:T], ALU.mult, [tm.T, bpt], [merged.T])
                    else:
                        tt(DVE, tm.t[:, 0:T], tm.t[:, 0:T], bp[:, 0:T], ALU.mult, [tm.T, bpt], [tm.T])
                        tt(DVE, merged.t[:, oc, 0:T], merged.t[:, oc, 0:T], tm.t[:, 0:T], ALU.add, [tm.T, merged.T], [merged.T])
            for half in range(2):
                wo, wot = ws_next()
                for q4 in range(4):
                    oc = half * 4 + q4
                    bo, bot = nb()
                    for kc in range(8):
                        mm(bo[:, 0:T], wo[:, kc, q4 * 128:(q4 + 1) * 128], merged.t[:, kc, 0:T], [wot, merged.T], [bot], start=(kc == 0), stop=(kc == 7))
                    act(yT.t[:, oc, 0:T], bo[:, 0:T], AF.Copy, [bot], [yT.T])
            postnorm(l, sqi, 1, T)

        def run_tile(is_prompt, seq, tj, T):
            sqi = seq if is_prompt else NPS + seq
            src = xp[seq] if is_prompt else xs[seq]
            dst = yp[seq] if is_prompt else ys[seq]
            NB = max(1, T // 128)
            tb = min(T, 128)
            phase("io")
            k.dma(POOL, xtok.t[0:tb, 0:NB, :], src[tj * 512:tj * 512 + T, :].rearrange("(b p) f -> p b f", p=tb), W=[xtok.T])
            for c in range(8):
                bt, btr = nb()
                for b in range(NB):
                    trp(bt[:, b * 128:b * 128 + tb], xtok.t[0:tb, b, c * 128:(c + 1) * 128], ident[0:tb, 0:tb], [xtok.T, cst.T], [btr])
                if c % 2:
                    cp(DVE, xT.t[:, c, 0:T], bt[:, 0:T], [btr], [xT.T])
                else:
                    act(xT.t[:, c, 0:T], bt[:, 0:T], AF.Copy, [btr], [xT.T])
            stop_at(1)
            tap(0, xT.t[:, :, :].rearrange("p c t -> p (c t)"), xT.T, 4096)
            tap(5, coef.t[:, :, :, :, :].rearrange("p a b c d -> p (a b c d)"), coef.T, NL * NSQ * 72)
            for l in range(NL):
                ffn(l, sqi, 0, T)
                tap(3, xT.t[:, :, :].rearrange("p c t -> p (c t)"), xT.T, 4096)
                stop_at(2)
                mixer(l, sqi, T, is_prompt, seq, tj)
                ffn(l, sqi, 2, T)
            phase("io")
            for b in range(NB):
                for half in range(2):
                    bt, btr = nb()
                    for c4 in range(4):
                        c = half * 4 + c4
                        trp(bt[0:tb, c4 * 128:(c4 + 1) * 128], xT.t[:, c, b * 128:b * 128 + tb], ident, [xT.T, cst.T], [btr])
                    cp(DVE, xtok.t[0:tb, b, half * 512:(half + 1) * 512], bt[0:tb, :], [btr], [xtok.T])
            k.dma(POOL, dst[tj * 512:tj * 512 + T, :].rearrange("(b p) f -> p b f", p=tb), xtok.t[0:tb, 0:NB, :], R=[xtok.T], prim=xtok.T, is_output=True)

        class StopBuild(Exception):
            pass

        def stop_at(level):
            if cfg.get("stop", 99) == level:
                raise StopBuild()
        try:
            stop_at(0)
            for seq in range(NPS):
                for tj in range(NTILE):
                    run_tile(True, seq, tj, 512)
            if not cfg.get("skip_sample", False):
                for seq in range(NSS):
                    run_tile(False, seq, 0, DEC_SEQ)
        except StopBuild:
            pass

        k.finish()
        k.replay()
        build.n_inst = k.n_inst
    return nc


_W_NAMES = ["w_ada", "b_ada", "norm_g", "w_ffn1", "w_ffn3", "w_ffn2", "w_in", "b_if", "conv_w", "conv_b", "lam", "norm_a", "norm_m", "w_pa", "w_pb", "w_out"]


def run(inputs, cfg, n_cores=8):
    NPS = cfg.get("n_pseq", 2)
    NSS = cfg.get("n_sseq", 2)
    nc = build(cfg)
    consts = make_consts()
    f = lambda a: np.ascontiguousarray(np.asarray(a, dtype=np.float32))
    wts = {n: f(inputs[n]) for n in _W_NAMES}
    in_maps = []
    for c in range(n_cores):
        ps = slice(c * NPS, (c + 1) * NPS)
        ss = slice(c * NSS, (c + 1) * NSS)
        m = dict(wts)
        m["xp"] = f(inputs["x_prompt"][ps]); m["xs"] = f(inputs["x_sample"][ss])
        m["cc"] = f(np.concatenate([inputs["c_prompt"][ps], inputs["c_sample"][ss]], axis=0))
        m["cache_k"] = f(np.asarray(inputs["cache_k"])[:, ss].reshape(-1, NSS, PAST, 512))
        m["cache_v"] = f(np.asarray(inputs["cache_v"])[:, ss].reshape(-1, NSS, PAST, 512))
        m["state_c"] = f(np.asarray(inputs["state_c"])[:, ss]); m["state_n"] = f(np.asarray(inputs["state_n"])[:, ss])
        m["state_m"] = f(np.asarray(inputs["state_m"])[:, ss]); m["state_conv"] = f(np.asarray(inputs["state_conv"])[:, ss])
        m["consts"] = consts
        in_maps.append(m)
    res = run_bass_kernel_spmd(nc, in_maps, core_ids=list(range(n_cores)))
    R = res.results
    if cfg.get("dbg", False):
        run.dbg = R[0]["dbg"]
    cat0 = lambda n: np.concatenate([r[n] for r in R], axis=0)
    cat1 = lambda n: np.concatenate([r[n] for r in R], axis=1)
    nl = cfg.get("depth", DEPTH)
    sl = cfg.get("seq", SEQ)
    out = (cat0("yp"), cat0("ys"),
           cat1("pk").reshape(nl, -1, sl, 4, 128), cat1("pv").reshape(nl, -1, sl, 4, 128),
           cat1("pc"), cat1("pn"), cat1("pm"), cat1("pconv"),
           cat1("sk").reshape(nl, -1, DEC_SEQ, 4, 128), cat1("sv").reshape(nl, -1, DEC_SEQ, 4, 128),
           cat1("sc"), cat1("sn"), cat1("sm"), cat1("sconv"))
    return tuple(np.ascontiguousarray(o.astype(np.float32)) for o in out)


def kernel(**inputs):
    return run(inputs, {})
```
